# Optimizing a Trainium2 kernel written in Bass

```python
import math
import jax, jax.numpy as jnp
from jax import lax
import numpy as np

D_MODEL = 2048
BATCH = 1
SEQ = 8192
DEPTH = 4

M_HEADS = 4
M_WIDTH = D_MODEL // 2
M_V_DIM = M_WIDTH // M_HEADS
M_QK_DIM = M_V_DIM // 2
M_CHUNK = 64
CONV_WIDTH = 5
A_HEADS = 8
A_WIDTH = D_MODEL // 2
A_V_DIM = A_WIDTH // A_HEADS
A_HEAD_DIM = A_V_DIM // 2
ROPE_THETA = 500000.0
ROPE_DIM = A_HEAD_DIM // 4
Q_BLOCK = 128
N_GROUPS = 4
EXPERTS_PER_GROUP = 8
N_EXPERTS = N_GROUPS * EXPERTS_PER_GROUP
TOP_K = 2
D_EXPERT = D_MODEL // 4
EPS = 1e-6

COL_SIZES = (
    M_HEADS * M_QK_DIM,
    M_HEADS * M_QK_DIM,
    M_WIDTH,
    4 * M_HEADS,
    M_WIDTH,
    A_HEADS * 2 * A_HEAD_DIM,
    A_HEADS * 2 * A_HEAD_DIM,
    A_WIDTH,
    D_MODEL,
    D_MODEL,
)
N_IN = sum(COL_SIZES)

kernel_name = "hybrid_mlstm_diffattn_hmoe_encoder"


def rms_norm(x, g=None):
    xf = x.astype(jnp.float32)
    y = xf * lax.rsqrt(jnp.mean(xf * xf, axis=-1, keepdims=True) + EPS)
    if g is not None:
        y = y * g.astype(jnp.float32)
    return y.astype(x.dtype)


def rope_tables(seq):
    pos = jnp.arange(seq, dtype=jnp.float32)
    inv = ROPE_THETA ** (-jnp.arange(0, ROPE_DIM, 2, dtype=jnp.float32) / ROPE_DIM)
    ang = pos[:, None] * inv[None, :]
    return jnp.cos(ang), jnp.sin(ang)


def partial_rope(x, cos, sin):
    half = ROPE_DIM // 2
    x1 = x[..., :half].astype(jnp.float32)
    x2 = x[..., half:ROPE_DIM].astype(jnp.float32)
    rot = jnp.concatenate([x1 * cos - x2 * sin, x2 * cos + x1 * sin], axis=-1).astype(x.dtype)
    return jnp.concatenate([rot, x[..., ROPE_DIM:]], axis=-1)


def centred_depthwise_conv(x, w):
    pad = w.shape[0] // 2
    return lax.conv_general_dilated(
        x, w[:, None, :].astype(x.dtype), window_strides=(1,), padding=[(pad, pad)],
        dimension_numbers=("NWC", "WIO", "NWC"), feature_group_count=x.shape[-1])


def mlstm_chunkwise(q, k, v, log_i, log_f):
    B, H, S, dk = q.shape
    dv = v.shape[-1]
    nc = S // M_CHUNK

    def to_chunks(t):
        t = t.reshape((B, H, nc, M_CHUNK) + t.shape[3:])
        return jnp.moveaxis(t, 2, 0)

    scan_order = jnp.tril(jnp.ones((M_CHUNK, M_CHUNK), dtype=bool))

    def step(carry, xs):
        C, n, m = carry
        qc, kc, vc, ic, fc = xs
        b = jnp.cumsum(fc, axis=-1)
        dmat = b[..., :, None] - b[..., None, :] + ic[..., None, :]
        dmat = jnp.where(scan_order, dmat, -jnp.inf)
        m_inter = b + m[..., None]
        m_t = jnp.maximum(m_inter, jnp.max(dmat, axis=-1))
        s = jnp.einsum('bhld,bhsd->bhls', qc, kc) * jnp.exp(dmat - m_t[..., None])
        inter = jnp.exp(m_inter - m_t)
        num = inter[..., None] * jnp.einsum('bhld,bhdv->bhlv', qc, C) + jnp.einsum('bhls,bhsv->bhlv', s, vc)
        den = inter * jnp.einsum('bhld,bhd->bhl', qc, n) + jnp.sum(s, axis=-1)
        h = num / jnp.maximum(jnp.abs(den), jnp.exp(-m_t))[..., None]
        b_end = b[..., -1]
        g = b_end[..., None] - b + ic
        m_new = jnp.maximum(b_end + m, jnp.max(g, axis=-1))
        w = jnp.exp(g - m_new[..., None])
        decay = jnp.exp(b_end + m - m_new)
        C = decay[..., None, None] * C + jnp.einsum('bhs,bhsd,bhsv->bhdv', w, kc, vc)
        n = decay[..., None] * n + jnp.einsum('bhs,bhsd->bhd', w, kc)
        return (C, n, m_new), h

    init = (jnp.zeros((B, H, dk, dv), jnp.float32), jnp.zeros((B, H, dk), jnp.float32),
            jnp.zeros((B, H), jnp.float32))
    _, h = lax.scan(step, init, (to_chunks(q), to_chunks(k), to_chunks(v),
                                 to_chunks(log_i), to_chunks(log_f)))
    return jnp.moveaxis(h, 0, 2).reshape(B, H, S, dv)


def mlstm_branch(zq, zk, zv, zg, zo, conv_w, gate_b, norm_g):
    B, S, _ = zq.shape
    dtype = zq.dtype
    qk = jax.nn.silu(centred_depthwise_conv(jnp.concatenate([zq, zk], axis=-1), conv_w))
    zq, zk = jnp.split(qk, 2, axis=-1)
    heads = lambda t, d: t.reshape(B, S, M_HEADS, d).transpose(0, 2, 1, 3).astype(jnp.float32)
    q = heads(zq, M_QK_DIM) * (M_QK_DIM ** -0.5)
    k = heads(zk, M_QK_DIM)
    v = heads(zv, M_V_DIM)
    gates = (zg.reshape(B, S, 4, M_HEADS) + gate_b).astype(jnp.float32).transpose(2, 0, 3, 1)
    i_fwd, f_fwd, i_bwd, f_bwd = gates[0], gates[1], gates[2], gates[3]
    h_fwd = mlstm_chunkwise(q, k, v, i_fwd, jax.nn.log_sigmoid(f_fwd))
    flip = lambda t: jnp.flip(t, axis=2)
    h_bwd = flip(mlstm_chunkwise(flip(q), flip(k), flip(v), flip(i_bwd), flip(jax.nn.log_sigmoid(f_bwd))))
    h = (h_fwd + h_bwd).transpose(0, 2, 1, 3)
    h = rms_norm(h).reshape(B, S, M_WIDTH) * norm_g.astype(jnp.float32)
    return (h * jax.nn.sigmoid(zo.astype(jnp.float32))).astype(dtype)


def diff_attention_branch(zq, zk, zv, qn_g, kn_g, lam, subln_g, lam_init, cos, sin):
    B, S, _ = zq.shape
    nb = S // Q_BLOCK
    q = zq.reshape(B, S, A_HEADS, 2, A_HEAD_DIM).transpose(0, 2, 3, 1, 4)
    k = zk.reshape(B, S, A_HEADS, 2, A_HEAD_DIM).transpose(0, 2, 3, 1, 4)
    v = zv.reshape(B, S, A_HEADS, A_V_DIM).transpose(0, 2, 1, 3)
    q = partial_rope(rms_norm(q, qn_g), cos, sin) * (A_HEAD_DIM ** -0.5)
    k = partial_rope(rms_norm(k, kn_g), cos, sin)
    lamf = lam.astype(jnp.float32)
    lam_full = jnp.exp(jnp.sum(lamf[0] * lamf[1])) - jnp.exp(jnp.sum(lamf[2] * lamf[3])) + lam_init
    qb = q.reshape(B, A_HEADS, 2, nb, Q_BLOCK, A_HEAD_DIM).transpose(3, 0, 1, 2, 4, 5)

    def block(q_blk):
        s = jnp.einsum('bhcqd,bhckd->bhcqk', q_blk, k).astype(jnp.float32)
        p = jax.nn.softmax(s, axis=-1)
        a = p[:, :, 0] - lam_full * p[:, :, 1]
        return jnp.einsum('bhqk,bhkv->bhqv', a.astype(v.dtype), v)

    o = lax.map(block, qb)
    o = o.transpose(1, 0, 3, 2, 4).reshape(B, S, A_HEADS, A_V_DIM)
    o = rms_norm(o, subln_g) * (1.0 - lam_init)
    return o.reshape(B, S, A_WIDTH)


def hierarchical_moe(h, rg_w, rg_b, re_w, re_b, w_gate, w_up, w_down):
    B, S, D = h.shape
    t = h.reshape(B * S, D)
    g_logits = (t @ rg_w + rg_b).astype(jnp.float32)
    g_val, g_idx = lax.top_k(jax.nn.softmax(g_logits, axis=-1), 1)
    e_logits = (t @ re_w + re_b).astype(jnp.float32).reshape(-1, N_GROUPS, EXPERTS_PER_GROUP)
    within = jnp.einsum('tg,tge->te', jax.nn.one_hot(g_idx[:, 0], N_GROUPS, dtype=jnp.float32), e_logits)
    e_val, e_idx = lax.top_k(within, TOP_K)
    e_w = jax.nn.softmax(e_val, axis=-1) * g_val
    global_idx = g_idx * EXPERTS_PER_GROUP + e_idx
    combine = jnp.einsum('tk,tke->te', e_w, jax.nn.one_hot(global_idx, N_EXPERTS, dtype=jnp.float32))
    hg = jnp.einsum('td,edf->tef', t, w_gate)
    hu = jnp.einsum('td,edf->tef', t, w_up)
    a = jax.nn.silu(hg) * hu * combine.astype(t.dtype)[:, :, None]
    y = jnp.einsum('tef,efd->td', a, w_down)
    return y.reshape(B, S, D)


def setup_inputs(seed: int = 0) -> dict:
    key = jax.random.key(seed)
    ks = jax.random.split(key, 32)
    nrm = lambda k, shape, scale: jax.random.normal(k, shape, jnp.float32) * scale
    L, D = DEPTH, D_MODEL
    f_base = jnp.linspace(3.0, 6.0, M_HEADS, dtype=jnp.float32)
    zero_h = jnp.zeros((M_HEADS,), jnp.float32)
    gate_base = jnp.stack([zero_h, f_base, zero_h, f_base])
    return {
        "x": nrm(ks[0], (BATCH, SEQ, D), 1.0),
        "c": nrm(ks[1], (BATCH, D), 1.0),
        "ada_w": nrm(ks[2], (L, D, 6 * D), 0.5 * D ** -0.5),
        "ada_b": nrm(ks[3], (L, 6 * D), 0.02),
        "norm1_g": 1.0 + nrm(ks[4], (L, D), 0.02),
        "norm2_g": 1.0 + nrm(ks[5], (L, D), 0.02),
        "w_in": nrm(ks[6], (L, D, N_IN), D ** -0.5),
        "m_conv_w": nrm(ks[7], (L, CONV_WIDTH, 2 * M_HEADS * M_QK_DIM), CONV_WIDTH ** -0.5),
        "m_gate_b": gate_base[None] + nrm(ks[8], (L, 4, M_HEADS), 0.1),
        "m_norm_g": 1.0 + nrm(ks[9], (L, M_WIDTH), 0.02),
        "a_qnorm_g": 1.0 + nrm(ks[10], (L, A_HEAD_DIM), 0.02),
        "a_knorm_g": 1.0 + nrm(ks[11], (L, A_HEAD_DIM), 0.02),
        "a_lambda": nrm(ks[12], (L, 4, A_HEAD_DIM), 0.1),
        "a_subln_g": 1.0 + nrm(ks[13], (L, A_V_DIM), 0.02),
        "w_branch_m": nrm(ks[14], (L, M_WIDTH, D), M_WIDTH ** -0.5),
        "w_branch_a": nrm(ks[15], (L, A_WIDTH, D), A_WIDTH ** -0.5),
        "w_out": nrm(ks[16], (L, D, D), D ** -0.5),
        "rg_w": nrm(ks[17], (L, D, N_GROUPS), D ** -0.5),
        "rg_b": nrm(ks[18], (L, N_GROUPS), 0.01),
        "re_w": nrm(ks[19], (L, D, N_EXPERTS), D ** -0.5),
        "re_b": nrm(ks[20], (L, N_EXPERTS), 0.01),
        "e_w_gate": nrm(ks[21], (L, N_EXPERTS, D, D_EXPERT), D ** -0.5),
        "e_w_up": nrm(ks[22], (L, N_EXPERTS, D, D_EXPERT), D ** -0.5),
        "e_w_down": nrm(ks[23], (L, N_EXPERTS, D_EXPERT, D), D_EXPERT ** -0.5),
    }


def reference(x, c, ada_w, ada_b, norm1_g, norm2_g, w_in, m_conv_w, m_gate_b, m_norm_g,
              a_qnorm_g, a_knorm_g, a_lambda, a_subln_g, w_branch_m, w_branch_a, w_out,
              rg_w, rg_b, re_w, re_b, e_w_gate, e_w_up, e_w_down):
    B, S, D = x.shape
    cos, sin = rope_tables(S)
    c_act = jax.nn.silu(c)
    split_at = [int(v) for v in np.cumsum(COL_SIZES)[:-1]]
    for l in range(DEPTH):
        lam_init = 0.8 - 0.6 * math.exp(-0.3 * l)
        mod = (c_act @ ada_w[l] + ada_b[l])[:, None, :]
        sh1, sc1, g1, sh2, sc2, g2 = jnp.split(mod, 6, axis=-1)
        h = rms_norm(x, norm1_g[l]) * (1.0 + sc1) + sh1
        z = h @ w_in[l]
        mq, mk, mv, mg, mo, aq, ak, av, gm, ga = jnp.split(z, split_at, axis=-1)
        y_m = mlstm_branch(mq, mk, mv, mg, mo, m_conv_w[l], m_gate_b[l], m_norm_g[l])
        y_a = diff_attention_branch(aq, ak, av, a_qnorm_g[l], a_knorm_g[l], a_lambda[l],
                                    a_subln_g[l], lam_init, cos, sin)
        merged = jax.nn.sigmoid(gm) * (y_m @ w_branch_m[l]) + jax.nn.sigmoid(ga) * (y_a @ w_branch_a[l])
        x = x + g1 * (merged @ w_out[l])
        h2 = rms_norm(x, norm2_g[l]) * (1.0 + sc2) + sh2
        x = x + g2 * hierarchical_moe(h2, rg_w[l], rg_b[l], re_w[l], re_b[l],
                                      e_w_gate[l], e_w_up[l], e_w_down[l])
    return x
```

```python
import contextlib
import math
import numpy as np
import concourse.bass as bass
import concourse.mybir as mybir
from concourse.bass_utils import run_bass_kernel_spmd

F32 = mybir.dt.float32
BF16 = mybir.dt.bfloat16
I32 = mybir.dt.int32
AF = mybir.ActivationFunctionType
ALU = mybir.AluOpType

NCORES = 8
D = 2048
S = 8192
TPC = S // NCORES
DEPTH = 4
KC = D // 128
EPS = 1e-6
N_IN = 10256
C_MQ, C_MK, C_MV, C_MG, C_MO, C_AQ, C_AK, C_AV, C_GM, C_GA = 0, 512, 1024, 2048, 2064, 3088, 4112, 5136, 6160, 8208


class Prog:
    COMPUTE = ("pe", "act", "dve", "pool")

    def __init__(self):
        self.nc = bass.Bass("TRN2", target_bir_lowering=False)
        self.stack = contextlib.ExitStack()
        self.ops = {e: [] for e in ("pe", "act", "dve", "pool", "sp")}
        self.cnt = {e: 0 for e in self.COMPUTE}
        self.last_w = {}
        self.readers = {}
        self.waited = {e: {} for e in self.ops}
        self.dma_cnt = {}
        self.semnames = ["c_" + e for e in self.COMPUTE]
        self.out_tokens = []
        self._n = 0
        self.cur = self.stack
        self.pending = {e: {} for e in self.ops}

    def dram(self, name, shape, dtype, kind):
        return self.nc.dram_tensor(name, list(shape), dtype, kind=kind).ap()

    def sbuf(self, shape, dtype, name=None):
        self._n += 1
        return self.cur.enter_context(self.nc.sbuf_tensor(name or f"sb{self._n}", list(shape), dtype))

    def psum(self, shape, dtype=F32, name=None):
        self._n += 1
        return self.cur.enter_context(self.nc.psum_tensor(name or f"ps{self._n}", list(shape), dtype))

    @contextlib.contextmanager
    def scope(self):
        prev = self.cur
        self.cur = contextlib.ExitStack()
        try:
            yield
        finally:
            self.cur.close()
            self.cur = prev
            self.fence()

    def fence(self):
        toks = {"c_" + e: self.cnt[e] for e in self.COMPUTE if self.cnt[e] > 0}
        toks.update({sn: v for sn, v in self.dma_cnt.items() if v > 0})
        for e in self.ops:
            self.pending[e] = dict(toks)

    def _deps(self, eng, reads, writes):
        deps = set()
        for k in reads:
            t = self.last_w.get(k)
            if t is not None:
                deps.add(t)
        for k in writes:
            t = self.last_w.get(k)
            if t is not None:
                deps.add(t)
            for r in self.readers.get(k, ()):
                deps.add(r)
        if self.pending[eng]:
            deps |= set(self.pending[eng].items())
            self.pending[eng] = {}
        waits = []
        for (sn, val) in deps:
            if sn == "c_pe" and eng == "pe":
                continue
            if sn in self.dma_cnt:
                val = self.dma_cnt[sn]
            if self.waited[eng].get(sn, 0) >= val:
                continue
            waits.append((sn, val))
        best = {}
        for sn, val in waits:
            best[sn] = max(best.get(sn, 0), val)
        for sn, val in best.items():
            self.waited[eng][sn] = val
        return sorted(best.items())

    def _commit(self, tok, reads, writes):
        for k in writes:
            self.last_w[k] = tok
            self.readers[k] = []
        for k in reads:
            if k in writes:
                continue
            self.readers.setdefault(k, []).append(tok)

    def op(self, eng, fn, reads=(), writes=()):
        reads, writes = tuple(reads), tuple(writes)
        waits = self._deps(eng, reads, writes)
        self.cnt[eng] += 1
        tok = ("c_" + eng, self.cnt[eng])
        self.ops[eng].append((waits, fn, tok[0], 1))
        self._commit(tok, reads, writes)
        return tok

    def dma(self, q, out, in_, sem, reads=(), writes=(), is_out=False, fn=None):
        reads, writes = tuple(reads), tuple(writes)
        if q == "pool":
            self._pq = getattr(self, "_pq", 0) + 1
            writes = writes + (("_poolq", self._pq % 4),)
        waits = self._deps(q, reads, writes)
        if sem not in self.dma_cnt:
            self.dma_cnt[sem] = 0
            self.semnames.append(sem)
        self.dma_cnt[sem] += 16
        tok = (sem, self.dma_cnt[sem])
        if fn is None:
            fn = lambda e, o=out, i=in_: e.dma_start(out=o, in_=i)
        self.ops[q].append((waits, fn, sem, 16))
        self._commit(tok, reads, writes)
        if is_out:
            self.out_tokens.append(tok)
        return tok

    def mm(self, out, lhsT, rhs, start, stop, reads, writes):
        return self.op("pe", lambda e: e.matmul(out, lhsT, rhs, start=start, stop=stop), reads, writes)

    def act(self, out, in_, func, reads, writes, bias=None, scale=None, eng="act"):
        kw = {}
        if bias is not None:
            kw["bias"] = bias
        if scale is not None:
            kw["scale"] = scale
        return self.op(eng, lambda e: e.activation(out=out, in_=in_, func=func, **kw), reads, writes)

    def ts(self, eng, out, in0, s1, s2, op0, op1, reads, writes):
        if s2 is None:
            return self.op(eng, lambda e: e.tensor_scalar(out=out, in0=in0, scalar1=s1, scalar2=None, op0=op0), reads, writes)
        return self.op(eng, lambda e: e.tensor_scalar(out=out, in0=in0, scalar1=s1, scalar2=s2, op0=op0, op1=op1), reads, writes)

    def stt(self, eng, out, in0, scalar, in1, op0, op1, reads, writes):
        return self.op(eng, lambda e: e.scalar_tensor_tensor(out=out, in0=in0, scalar=scalar, in1=in1, op0=op0, op1=op1), reads, writes)

    def tt(self, eng, out, in0, in1, op, reads, writes):
        return self.op(eng, lambda e: e.tensor_tensor(out=out, in0=in0, in1=in1, op=op), reads, writes)

    def copy(self, eng, out, in_, reads, writes):
        if eng == "act":
            return self.op(eng, lambda e: e.activation(out=out, in_=in_, func=AF.Copy), reads, writes)
        return self.op(eng, lambda e: e.tensor_copy(out=out, in_=in_), reads, writes)

    def memset(self, eng, ap, val, writes):
        return self.op(eng, lambda e: e.memset(ap, val), (), writes)

    def build(self):
        nc = self.nc
        fin = {}
        for sn, val in self.out_tokens:
            fin[sn] = max(fin.get(sn, 0), val)
        sems = {}
        for sn in self.semnames:
            sems[sn] = self.stack.enter_context(nc.semaphore(sn))
        ops = self.ops

        def emit(name, e):
            for waits, fn, sn, inc in ops[name]:
                for wsn, val in waits:
                    e.wait_ge(sems[wsn], val)
                fn(e).then_inc(sems[sn], inc)

        with nc.Block() as block:
            @block.tensor
            def _(e):
                emit("pe", e)

            @block.scalar
            def _(e):
                emit("act", e)

            @block.vector
            def _(e):
                emit("dve", e)

            @block.gpsimd
            def _(e):
                emit("pool", e)

            @block.sync
            def _(e):
                emit("sp", e)
                for sn, val in sorted(fin.items()):
                    e.wait_ge(sems[sn], val)
        self.stack.close()
        return nc


def _run(prog, in_maps):
    nc = prog.build()
    res = run_bass_kernel_spmd(nc, in_maps, core_ids=list(range(NCORES)))
    return res.results


def _pk(v):
    v = np.asarray(v, np.float32).reshape(-1, 128)
    return np.ascontiguousarray(v.T)


MODC = 6 * D // NCORES


def build_mod():
    p = Prog()
    c_in = p.dram("c_pk", [128, KC], F32, "ExternalInput")
    w_in = p.dram("ada_w", [DEPTH * 3, 128, KC, 512], F32, "ExternalInput")
    b_in = p.dram("ada_b", [1, DEPTH * MODC], F32, "ExternalInput")
    out = p.dram("mod", [1, DEPTH * MODC], F32, "ExternalOutput")
    c_sb = p.sbuf([128, KC], F32)
    ca = p.sbuf([128, KC], F32)
    b_sb = p.sbuf([1, DEPTH * MODC], F32)
    o_sb = p.sbuf([1, DEPTH * MODC], F32)
    wbuf = [p.sbuf([128, KC, 512], F32) for _ in range(3)]
    ps = [p.psum([128, 512]) for _ in range(2)]
    p.dma("sp", c_sb[:, :], c_in[:, :], "ld_c", writes=["c"])
    p.dma("sp", b_sb[:, :], b_in[:, :], "ld_b", writes=["b"])
    p.act(ca[:, :], c_sb[:, :], AF.Silu, ["c"], ["ca"])
    for j in range(DEPTH * 3):
        wb = wbuf[j % 3]
        p.dma("sp" if j % 2 == 0 else "pool", wb[:, :, :], w_in[j], f"ld_w{j % 3}", writes=[("w", j % 3)])
        pj = ps[j % 2]
        for k in range(KC):
            p.mm(pj[0:1, :], ca[:, k:k + 1], wb[:, k, :], k == 0, k == KC - 1, ["ca", ("w", j % 3)], [("ps", j % 2)])
        p.tt("dve", o_sb[0:1, j * 512:(j + 1) * 512], pj[0:1, :], b_sb[0:1, j * 512:(j + 1) * 512], ALU.add,
             [("ps", j % 2), "b"], [("o", j)])
    p.dma("sp", out[:, :], o_sb[:, :], "st_o", reads=[("o", j) for j in range(DEPTH * 3)], is_out=True)
    return p


def run_mod(c, ada_w, ada_b):
    c_pk = _pk(c.reshape(-1))
    in_maps = []
    for i in range(NCORES):
        w = ada_w[:, :, i * MODC:(i + 1) * MODC]
        w = w.reshape(DEPTH, KC, 128, 3, 512).transpose(0, 3, 2, 1, 4)
        in_maps.append({"c_pk": c_pk, "ada_w": np.ascontiguousarray(w.reshape(DEPTH * 3, 128, KC, 512)),
                        "ada_b": np.ascontiguousarray(ada_b[:, i * MODC:(i + 1) * MODC].reshape(1, -1))})
    res = _run(build_mod(), in_maps)
    mod = np.concatenate([r["mod"].reshape(DEPTH, MODC) for r in res], axis=1)
    return mod


def _load_xT(p, xT_d, x_sb, q="sp"):
    for g in range(4):
        p.dma(q, x_sb[:, 4 * g:4 * g + 4, :], xT_d.rearrange("(k p) t -> p k t", p=128)[:, 4 * g:4 * g + 4, :],
              f"ld_x{g}", writes=[("x", k) for k in range(4 * g, 4 * g + 4)])


def _norm_mod(p, getx, hT, gpk, scpk, shpk, ones_f, ps_ss, tagp, hook=None, tagpar=None):
    T = TPC
    tagpar = tagpar or tagp
    gs = p.sbuf([128, KC], F32)
    p.stt("dve", gs[:, :], scpk, 1.0, gpk, ALU.add, ALU.mult, [tagpar], [tagp + "gs"])
    sq = [p.sbuf([128, T], F32) for _ in range(2)]
    for k in range(KC):
        s = sq[k % 2]
        xa, xk = getx(k, 0)
        p.act(s[:, :], xa, AF.Square, [xk], [(tagp + "sq", k % 2)])
        for h in range(2):
            p.mm(ps_ss[h][:, :], ones_f[:, :], s[:, h * 512:(h + 1) * 512], k == 0, k == KC - 1,
                 [(tagp + "sq", k % 2), "ones_f"], [(tagp + "ss", h)])
    rstd = p.sbuf([128, T], F32)
    for h in range(2):
        p.act(rstd[:, h * 512:(h + 1) * 512], ps_ss[h][:, :], AF.Sqrt, [(tagp + "ss", h)], [(tagp + "rstd", h)],
              bias=EPS, scale=1.0 / D)
        p.op("dve", lambda e, h=h: e.reciprocal(out=rstd[:, h * 512:(h + 1) * 512], in_=rstd[:, h * 512:(h + 1) * 512]),
             [(tagp + "rstd", h)], [(tagp + "rstd", h)])
    tmp = [p.sbuf([128, T], F32) for _ in range(2)]
    hf = sq
    for k in range(KC):
        t = tmp[k % 2]
        f = hf[k % 2]
        xa, xk = getx(k, 1)
        p.stt("dve", t[:, :], xa, gs[:, k:k + 1], rstd[:, :], ALU.mult, ALU.mult,
              [xk, tagp + "gs", (tagp + "rstd", 0), (tagp + "rstd", 1)], [(tagp + "tmp", k % 2)])
        p.act(f[:, :], t[:, :], AF.Identity, [(tagp + "tmp", k % 2), tagpar], [(tagp + "sq", k % 2)], bias=shpk[:, k:k + 1])
        p.copy("pool", hT[:, k, :], f[:, :], [(tagp + "sq", k % 2)], [(tagp + "h", k)])
        if hook is not None:
            hook(k, f, (tagp + "sq", k % 2))


NFM = 24
ZF_ROWS = NFM * 128 + 16


def build_A(combine):
    p = Prog()
    T = TPC
    xT_d = p.dram("xT", [D, T], F32, "ExternalInput")
    par_d = p.dram("par", [128, 4 * KC], F32, "ExternalInput")
    wfm_d = p.dram("wfm", [NFM, 128, KC, 128], F32, "ExternalInput")
    wg_d = p.dram("wg", [128, KC, 16], F32, "ExternalInput")
    wtm_d = p.dram("wtm", [4, 128, KC, 512], F32, "ExternalInput")
    zf_d = p.dram("zf", [ZF_ROWS, T], F32, "ExternalOutput")
    zv_d = p.dram("zv", [T, 2048], F32, "ExternalOutput")
    if combine:
        yT_d = p.dram("yT", [2, D, T], F32, "ExternalInput")
        xo_d = p.dram("xo", [D, T], F32, "ExternalOutput")

    x_sb = p.sbuf([128, KC, T], F32)
    hT = p.sbuf([128, KC, T], BF16)
    par = p.sbuf([128, 4 * KC], F32)
    ones_f = p.sbuf([128, 128], F32)
    ps_ss = [p.psum([128, 512]) for _ in range(2)]
    ps_mm = [p.psum([128, 512]) for _ in range(4)]
    p.memset("pool", ones_f[:, :], 1.0, ["ones_f"])
    p.dma("sp", par[:, :], par_d[:, :], "ld_par", writes=["par"])
    _load_xT(p, xT_d, x_sb)

    if combine:
        ybuf = [p.sbuf([128, 2, T], F32) for _ in range(2)]
        for k in range(KC):
            yb = ybuf[k % 2]
            p.dma("sp", yb[:, :, :], yT_d.rearrange("s (k p) t -> p k s t", p=128)[:, k, :, :], f"ld_y{k % 2}",
                  writes=[("y", k % 2)])
            p.tt("pool", yb[:, 0, :], yb[:, 0, :], yb[:, 1, :], ALU.add, [("y", k % 2)], [("y", k % 2)])
            p.stt("dve", x_sb[:, k, :], yb[:, 0, :], par[:, 3 * KC + k:3 * KC + k + 1], x_sb[:, k, :], ALU.mult, ALU.add,
                  [("y", k % 2), ("x", k), "par"], [("x", k)])
        for g in range(4):
            p.dma("sp", xo_d.rearrange("(k p) t -> p k t", p=128)[:, 4 * g:4 * g + 4, :], x_sb[:, 4 * g:4 * g + 4, :],
                  f"st_x{g}", reads=[("x", k) for k in range(4 * g, 4 * g + 4)], is_out=True)

    wg = p.sbuf([128, KC, 16], F32)
    p.dma("sp", wg[:, :, :], wg_d[:, :, :], "ld_wg", writes=["wg"])
    ps_g = [p.psum([128, 512]) for _ in range(2)]

    def gate_hook(k, f, fkey):
        for h in range(2):
            p.mm(ps_g[h][0:16, :], wg[:, k, :], f[:, h * 512:(h + 1) * 512], k == 0, k == KC - 1, ["wg", fkey], [("psg", h)])

    _norm_mod(p, lambda k, ps_: (x_sb[:, k, :], ("x", k)), hT, par[:, 0:KC], par[:, KC:2 * KC], par[:, 2 * KC:3 * KC],
              ones_f, ps_ss, "par", hook=gate_hook)
    hkeys = [("parh", k) for k in range(KC)]

    wfm = [p.sbuf([128, KC, 128], BF16) for _ in range(3)]
    stg = [p.sbuf([128, T], F32) for _ in range(2)]
    mmi = 0
    for b in range(NFM + 1):
        isg = b == NFM
        M = 16 if isg else 128
        if not isg:
            w = wfm[b % 3]
            p.dma("pool", w[:, :, :], wfm_d[b], f"ld_wfm{b % 3}", writes=[("wfm", b % 3)])
            wkey = ("wfm", b % 3)
        st = stg[b % 2]
        for h in range(2):
            if isg:
                ps, pk = ps_g[h], ("psg", h)
            else:
                ps = ps_mm[mmi % 2]
                pk = ("psmm", mmi % 2)
                mmi += 1
                for k in range(KC):
                    p.mm(ps[0:M, :], w[:, k, :], hT[:, k, h * 512:(h + 1) * 512], k == 0, k == KC - 1, [wkey, hkeys[k]], [pk])
            p.copy("act" if h == 0 else "dve", st[0:M, h * 512:(h + 1) * 512], ps[0:M, :], [pk], [("stg", b % 2, h)])
        p.dma("sp", zf_d[b * 128:b * 128 + M, :], st[0:M, :], f"st_zf{b % 2}",
              reads=[("stg", b % 2, 0), ("stg", b % 2, 1)], is_out=True)

    wtm = [p.sbuf([128, KC, 512], BF16) for _ in range(2)]
    stv = [p.sbuf([128, 512], F32) for _ in range(3)]
    si = 0
    for s4 in range(4):
        w = wtm[s4 % 2]
        for g in range(4):
            p.dma("pool", w[:, 4 * g:4 * g + 4, :], wtm_d[s4][:, 4 * g:4 * g + 4, :], f"ld_wtm{s4 % 2}",
                  writes=[("wtm", s4 % 2, g)])
        for tt_ in range(T // 128):
            ps = ps_mm[mmi % 4]
            pk = ("psmm", mmi % 4)
            mmi += 1
            for k in range(KC):
                p.mm(ps[:, :], hT[:, k, tt_ * 128:(tt_ + 1) * 128], w[:, k, :], k == 0, k == KC - 1,
                     [("wtm", s4 % 2, k // 4), hkeys[k]], [pk])
            sv = stv[si % 3]
            p.copy("act" if si % 2 == 0 else "dve", sv[:, :], ps[:, :], [pk], [("stv", si % 3)])
            p.dma("sp", zv_d[tt_ * 128:(tt_ + 1) * 128, s4 * 512:(s4 + 1) * 512], sv[:, :], f"st_zv{si % 3}",
                  reads=[("stv", si % 3)], is_out=True)
            si += 1
    return p


def _blk(w, cols):
    return np.ascontiguousarray(w[:, cols].reshape(KC, 128, -1).transpose(1, 0, 2))


def prep_A_weights(w_in_l):
    fm_cols = []
    for h in range(4):
        fm_cols.append(np.arange(C_MQ + h * 128, C_MQ + (h + 1) * 128))
    for h in range(4):
        fm_cols.append(np.arange(C_MK + h * 128, C_MK + (h + 1) * 128))
    for h in range(8):
        fm_cols.append(np.arange(C_AQ + h * 128, C_AQ + (h + 1) * 128))
    for h in range(8):
        fm_cols.append(np.arange(C_AK + h * 128, C_AK + (h + 1) * 128))
    wfm = np.stack([_blk(w_in_l, c) for c in fm_cols])
    wg = _blk(w_in_l, np.arange(C_MG, C_MG + 16))
    tm = [np.arange(C_MV + j * 512, C_MV + (j + 1) * 512) for j in range(2)] + \
         [np.arange(C_AV + j * 512, C_AV + (j + 1) * 512) for j in range(2)]
    wtm = np.stack([_blk(w_in_l, c) for c in tm])
    return wfm, wg, wtm


def run_A(xT_sh, mod_l, norm1_g_l, w_in_l, g2prev=None, yT_sh=None):
    combine = yT_sh is not None
    sh1, sc1 = mod_l[0:D], mod_l[D:2 * D]
    par = np.concatenate([_pk(norm1_g_l), _pk(sc1), _pk(sh1), _pk(g2prev if combine else np.zeros(D, np.float32))], axis=1)
    wfm, wg, wtm = prep_A_weights(w_in_l)
    in_maps = []
    for i in range(NCORES):
        m = {"xT": xT_sh[i], "par": par, "wfm": wfm, "wg": wg, "wtm": wtm}
        if combine:
            m["yT"] = yT_sh[i]
        in_maps.append(m)
    res = _run(build_A(combine), in_maps)
    return [r["zf"] for r in res], [r["zv"] for r in res], ([r["xo"] for r in res] if combine else xT_sh)


def rope_tables_np():
    pos = np.arange(S, dtype=np.float32)
    inv = np.float32(500000.0) ** (-(np.arange(0, 16, 2, dtype=np.float32)) / np.float32(16))
    ang = pos[:, None] * inv[None, :].astype(np.float32)
    cos = np.cos(ang).astype(np.float32).T
    sin = np.sin(ang).astype(np.float32).T
    return np.ascontiguousarray(np.concatenate([cos, cos], 0)), np.ascontiguousarray(np.concatenate([sin, sin], 0))


def const_mats():
    blk64 = np.zeros((128, 128), np.float32)
    blk64[:64, :64] = 1
    blk64[64:, 64:] = 1
    pm = np.zeros((128, 128), np.float32)
    for base in (0, 64):
        for m in range(8):
            pm[base + m + 8, base + m] = -1.0
            pm[base + m, base + m + 8] = 1.0
    return blk64, pm


def _attention(p, d):
    QB = 512
    NQB = S // QB
    NKT = S // 128
    blk64 = p.sbuf([128, 128], F32)
    pm = p.sbuf([128, 128], F32)
    ones_f = p.sbuf([128, 128], F32)
    ones_bf = p.sbuf([128, 128], BF16)
    para = p.sbuf([128, 8], F32)
    lam = p.sbuf([128, 256], F32)
    sm = p.sbuf([128, 16], F32)
    p.dma("sp", blk64[:, :], d["blk64"][:, :], "ld_c0", writes=["blk64"])
    p.dma("sp", pm[:, :], d["pm"][:, :], "ld_c1", writes=["pm"])
    p.dma("sp", para[:, :], d["para"][:, :], "ld_c2", writes=["para"])
    p.dma("sp", lam[:, :], d["lam"][:, :], "ld_c3", writes=["lam"])
    p.memset("pool", ones_f[:, :], 1.0, ["ones_f"])
    p.memset("pool", ones_bf[:, :], 1.0, ["ones_bf"])
    lp = p.sbuf([128, 128], F32)
    p.tt("dve", lp[:, 0:64], lam[:, 0:64], lam[:, 64:128], ALU.mult, ["lam"], ["lp"])
    p.tt("dve", lp[:, 64:128], lam[:, 128:192], lam[:, 192:256], ALU.mult, ["lam", "lp"], ["lp"])
    p.op("dve", lambda e: e.reduce_sum(out=sm[:, 0:1], in_=lp[:, 0:64], axis=mybir.AxisListType.X), ["lp"], ["sm"])
    p.op("dve", lambda e: e.reduce_sum(out=sm[:, 1:2], in_=lp[:, 64:128], axis=mybir.AxisListType.X), ["lp", "sm"], ["sm"])
    p.act(sm[:, 2:4], sm[:, 0:2], AF.Exp, ["sm"], ["sm"])
    p.tt("dve", sm[:, 4:5], sm[:, 3:4], sm[:, 2:3], ALU.subtract, ["sm"], ["sm"])
    p.ts("dve", sm[:, 4:5], sm[:, 4:5], para[:, 3:4], None, ALU.add, None, ["sm", "para"], ["sm"])
    p.ts("dve", sm[:, 5:6], para[:, 0:1], 0.125, None, ALU.mult, None, ["para", "sm"], ["sm"])
    p.ts("dve", sm[:, 6:7], para[:, 1:2], 1.0, None, ALU.mult, None, ["para", "sm"], ["sm"])
    p.ts("dve", sm[:, 7:8], para[:, 2:3], para[:, 4:5], None, ALU.mult, None, ["para", "sm"], ["sm"])

    qn = p.sbuf([128, S], BF16)
    kn = p.sbuf([128, S], BF16)
    v_bf = p.sbuf([128, NKT, 128], BF16)
    for g in range(4):
        p.dma("pool", v_bf[:, 16 * g:16 * g + 16, :], d["av"].rearrange("(t p) c -> p t c", p=128)[:, 16 * g:16 * g + 16, :],
              "ld_v", writes=[("v", g)])
    psS = [[p.psum([128, 512]) for _ in range(2)] for _ in range(2)]
    psO = [p.psum([128, 512]) for _ in range(2)]
    psD = [p.psum([128, 512]) for _ in range(2)]
    skey = lambda c, i: ("psS", c, i)

    raw = [p.sbuf([128, QB], F32) for _ in range(2)]
    sqb = [p.sbuf([128, QB], F32) for _ in range(2)]
    rsb = [p.sbuf([128, QB], F32) for _ in range(2)]
    qnf = [p.sbuf([128, QB], F32) for _ in range(2)]
    cst = [p.sbuf([128, QB], F32) for _ in range(2)]
    snt = [p.sbuf([128, QB], F32) for _ in range(2)]
    t1b = [p.sbuf([128, QB], F32) for _ in range(2)]
    t2b = [p.sbuf([128, QB], F32) for _ in range(2)]
    it = 0
    for which, src_d, dst, gcol in (("q", d["aqT"], qn, 5), ("k", d["akT"], kn, 6)):
        for b in range(NQB):
            r = it % 2
            sl = slice(b * QB, (b + 1) * QB)
            p.dma("sp", raw[r][:, :], src_d[:, sl], f"ld_raw{r}", writes=[("raw", r)])
            for base in (0, 64):
                p.dma("sp", cst[r][base:base + 16, :], d["cos"][:, sl], f"ld_cs{r}", writes=[("cs", r, base)])
                p.dma("sp", snt[r][base:base + 16, :], d["sin"][:, sl], f"ld_sn{r}", writes=[("sn", r, base)])
            p.act(sqb[r][:, :], raw[r][:, :], AF.Square, [("raw", r)], [("sq", r)])
            p.mm(psS[0][r][:, :], blk64[:, :], sqb[r][:, :], True, True, ["blk64", ("sq", r)], [skey(0, r)])
            p.act(rsb[r][:, :], psS[0][r][:, :], AF.Sqrt, [skey(0, r)], [("rs", r)], bias=EPS, scale=1.0 / 64)
            p.op("dve", lambda e, r=r: e.reciprocal(out=rsb[r][:, :], in_=rsb[r][:, :]), [("rs", r)], [("rs", r)])
            p.stt("dve", qnf[r][:, :], raw[r][:, :], sm[:, gcol:gcol + 1], rsb[r][:, :], ALU.mult, ALU.mult,
                  [("raw", r), ("rs", r), "sm"], [("qnf", r)])
            p.mm(psS[1][r][:, :], pm[:, :], qnf[r][:, :], True, True, ["pm", ("qnf", r)], [skey(1, r)])
            p.copy("act", dst[:, sl], qnf[r][:, :], [("qnf", r)], [(which + "n", b)])
            for base in (0, 64):
                rows = slice(base, base + 16)
                p.tt("pool", t1b[r][rows, :], qnf[r][rows, :], cst[r][rows, :], ALU.mult,
                     [("qnf", r), ("cs", r, base)], [("t1", r, base)])
                p.tt("dve", t2b[r][rows, :], psS[1][r][rows, :], snt[r][rows, :], ALU.mult,
                     [skey(1, r), ("sn", r, base)], [("t2", r, base)])
                p.tt("dve", dst[rows, sl], t1b[r][rows, :], t2b[r][rows, :], ALU.add,
                     [("t1", r, base), ("t2", r, base), (which + "n", b)], [(which + "n", b)])
            it += 1

    NPT = 4
    PT = [[p.sbuf([128, QB], BF16) for _ in range(NPT)] for _ in range(2)]
    r1 = p.sbuf([128, QB], F32)
    r2 = p.sbuf([128, QB], F32)
    o1 = p.sbuf([128, QB], F32)
    o2 = p.sbuf([128, QB], F32)
    ob = [p.sbuf([128, QB], F32) for _ in range(2)]
    osq = p.sbuf([128, QB], F32)
    ors = p.sbuf([128, QB], F32)
    yst = [p.sbuf([128, QB], F32) for _ in range(2)]

    def qk(qb, kt):
        i = kt % 2
        for c in range(2):
            rows = slice(c * 64, (c + 1) * 64)
            p.mm(psS[c][i][:, :], kn[rows, kt * 128:(kt + 1) * 128], qn[rows, qb * QB:(qb + 1) * QB], True, True,
                 [("kn", kt // 4), ("qn", qb)], [skey(c, i)])

    step = 0
    for qb in range(NQB):
        qk(qb, 0)
        for kt in range(NKT):
            if kt + 1 < NKT:
                qk(qb, kt + 1)
            i = kt % 2
            sl = step % NPT
            step += 1
            for c in range(2):
                p.act(PT[c][sl][:, :], psS[c][i][:, :], AF.Exp, [skey(c, i)], [("PT", c, sl)])
            for c in range(2):
                p.mm(psO[c][:, :], v_bf[:, kt, :], PT[c][sl][:, :], kt == 0, kt == NKT - 1, [("v", kt // 16), ("PT", c, sl)], [("psO", c)])
                p.mm(psD[c][:, :], ones_bf[:, :], PT[c][sl][:, :], kt == 0, kt == NKT - 1, ["ones_bf", ("PT", c, sl)], [("psD", c)])
        o = ob[qb % 2]
        p.op("dve", lambda e: e.reciprocal(out=r1[:, :], in_=psD[0][:, :]), [("psD", 0)], ["r1"])
        p.op("dve", lambda e: e.reciprocal(out=r2[:, :], in_=psD[1][:, :]), [("psD", 1)], ["r2"])
        p.tt("dve", o1[:, :], psO[0][:, :], r1[:, :], ALU.mult, [("psO", 0), "r1"], ["o1"])
        p.tt("dve", o2[:, :], psO[1][:, :], r2[:, :], ALU.mult, [("psO", 1), "r2"], ["o2"])
        p.stt("dve", o[:, :], o2[:, :], sm[:, 4:5], o1[:, :], ALU.mult, ALU.add, ["o1", "o2", "sm"], [("o", qb % 2)])
        p.tt("pool", osq[:, :], o[:, :], o[:, :], ALU.mult, [("o", qb % 2)], ["osq"])
        p.mm(psS[0][0][:, :], ones_f[:, :], osq[:, :], True, True, ["ones_f", "osq"], [skey(0, 0)])
        p.act(ors[:, :], psS[0][0][:, :], AF.Sqrt, [skey(0, 0)], ["ors"], bias=EPS, scale=1.0 / 128)
        p.op("dve", lambda e: e.reciprocal(out=ors[:, :], in_=ors[:, :]), ["ors"], ["ors"])
        y = yst[qb % 2]
        p.stt("dve", y[:, :], o[:, :], sm[:, 7:8], ors[:, :], ALU.mult, ALU.mult, [("o", qb % 2), "sm", "ors"], [("y", qb % 2)])
        p.dma("sp", d["yaT"][:, qb * QB:(qb + 1) * QB], y[:, :], f"st_y{qb % 2}", reads=[("y", qb % 2)], is_out=True)


def build_B(do_attn=True, do_mlstm=True):
    p = Prog()
    d = {}
    if do_attn:
        d["aqT"] = p.dram("aqT", [128, S], F32, "ExternalInput")
        d["akT"] = p.dram("akT", [128, S], F32, "ExternalInput")
        d["av"] = p.dram("av", [S, 128], F32, "ExternalInput")
        d["para"] = p.dram("para", [128, 8], F32, "ExternalInput")
        d["lam"] = p.dram("lam", [128, 256], F32, "ExternalInput")
        d["cos"] = p.dram("cos", [16, S], F32, "ExternalInput")
        d["sin"] = p.dram("sin", [16, S], F32, "ExternalInput")
        d["blk64"] = p.dram("blk64", [128, 128], F32, "ExternalInput")
        d["pm"] = p.dram("pm", [128, 128], F32, "ExternalInput")
        d["yaT"] = p.dram("yaT", [128, S], F32, "ExternalOutput")
    if do_mlstm:
        _mlstm_decl(p, d)
        with p.scope():
            _mlstm(p, d)
    if do_attn:
        with p.scope():
            _attention(p, d)
    return p


def mlstm_consts():
    tri = np.triu(np.ones((128, 128), np.float32))
    maskp = np.where(np.arange(128)[:, None] <= np.arange(128)[None, :], 0.0, 30000.0).astype(np.float32)
    ident = np.eye(128, dtype=np.float32)
    return tri, maskp, ident


def _mlstm_decl(p, d):
    d["mqT"] = p.dram("mqT", [128, S], F32, "ExternalInput")
    d["mkT"] = p.dram("mkT", [128, S], F32, "ExternalInput")
    d["mv"] = p.dram("mv", [S, 256], F32, "ExternalInput")
    d["gtm"] = p.dram("gtm", [128, 128], F32, "ExternalInput")
    d["mpar"] = p.dram("mpar", [128, 12], F32, "ExternalInput")
    d["tri"] = p.dram("tri", [128, 128], F32, "ExternalInput")
    d["maskp"] = p.dram("maskp", [128, 128], F32, "ExternalInput")
    d["ident"] = p.dram("ident", [128, 128], F32, "ExternalInput")
    d["hmT"] = p.dram("hmT", [256, S], F32, "ExternalOutput")


def _scan_free(p, eng, bufs, n, op, key):
    cur, nxt = bufs[0], bufs[1]
    sh = 1
    while sh < n:
        p.tt(eng, nxt[:, sh:n], cur[:, sh:n], cur[:, 0:n - sh], op, [key], [key])
        p.copy(eng, nxt[:, 0:sh], cur[:, 0:sh], [key], [key])
        cur, nxt = nxt, cur
        sh *= 2
    return cur


def _mlstm(p, d):
    NT = S // 128
    LNS = math.log(128.0 ** -0.5)
    X = mybir.AxisListType.X
    mpar = p.sbuf([128, 12], F32)
    tri = p.sbuf([128, 128], F32)
    maskp = p.sbuf([128, 128], F32)
    ident = p.sbuf([128, 128], F32)
    ident_bf = p.sbuf([128, 128], BF16)
    ones_f = p.sbuf([128, 128], F32)
    ones_bf = p.sbuf([128, 128], BF16)
    gtm = p.sbuf([128, 128], F32)
    for nm, t in (("mpar", mpar), ("tri", tri), ("maskp", maskp), ("ident", ident), ("gtm", gtm)):
        p.dma("sp", t[:, :], d[nm][:, :], "ld_" + nm, writes=[nm])
    p.dma("pool", ident_bf[:, :], d["ident"][:, :], "ld_idbf", writes=["ident_bf"])
    p.memset("pool", ones_f[:, :], 1.0, ["ones_f"])
    p.memset("pool", ones_bf[:, :], 1.0, ["ones_bf"])
    v_bf = p.sbuf([128, NT, 256], BF16)
    for g in range(4):
        p.dma("pool", v_bf[:, 16 * g:16 * g + 16, :], d["mv"].rearrange("(t p) c -> p t c", p=128)[:, 16 * g:16 * g + 16, :],
              "ld_mv", writes=[("mv", g)])
    bank = [p.psum([128, 512]) for _ in range(6)]
    psT = [p.psum([128, 128], BF16) for _ in range(2)]

    i_tm = p.sbuf([128, 64], F32)
    lf = p.sbuf([128, 64], F32)
    p.ts("dve", i_tm[:, :], gtm[:, 0:64], mpar[:, 10:11], None, ALU.add, None, ["gtm", "mpar"], ["i_tm"])
    p.ts("dve", lf[:, :], gtm[:, 64:128], mpar[:, 11:12], None, ALU.add, None, ["gtm", "mpar"], ["lf"])
    p.act(lf[:, :], lf[:, :], AF.Exp, ["lf"], ["lf"], scale=-1.0)
    p.act(lf[:, :], lf[:, :], AF.Ln, ["lf"], ["lf"], bias=1.0)
    p.mm(bank[0][:, 0:64], tri[:, :], lf[:, :], True, True, ["tri", "lf"], [("bank", 0)])
    p.mm(bank[0][:, 64:128], ones_f[:, :], lf[:, :], True, True, ["ones_f", "lf"], [("bank", 0)])
    sc = [p.sbuf([128, 64], F32) for _ in range(2)]
    p.copy("dve", sc[0][:, :], bank[0][:, 64:128], [("bank", 0)], ["sc"])
    incl = _scan_free(p, "dve", sc, 64, ALU.add, "sc")
    nF = p.sbuf([128, 64], F32)
    p.tt("dve", nF[:, :], incl[:, :], bank[0][:, 64:128], ALU.subtract, ["sc", ("bank", 0)], ["nF"])
    p.tt("dve", nF[:, :], nF[:, :], bank[0][:, 0:64], ALU.add, ["nF", ("bank", 0)], ["nF"])
    a_tm = p.sbuf([128, 64], F32)
    p.tt("dve", a_tm[:, :], i_tm[:, :], nF[:, :], ALU.add, ["i_tm", "nF"], ["a_tm"])
    p.mm(bank[1][0:64, 0:128], a_tm[:, :], ident[:, :], True, True, ["a_tm", "ident"], [("bank", 1)])
    p.mm(bank[1][0:64, 128:256], nF[:, :], ident[:, :], True, True, ["nF", "ident"], [("bank", 1)])
    cm = [p.sbuf([64, 128], F32) for _ in range(2)]
    nF_G = p.sbuf([64, 128], F32)
    p.copy("dve", cm[0][:, :], bank[1][0:64, 0:128], [("bank", 1)], ["cm"])
    p.copy("dve", nF_G[:, :], bank[1][0:64, 128:256], [("bank", 1)], ["nF_G"])
    cmx = _scan_free(p, "dve", cm, 128, ALU.max, "cm")
    p.ts("dve", cmx[:, :], cmx[:, :], 0.0, None, ALU.max, None, ["cm"], ["cm"])
    p.mm(bank[2][0:1, 0:64], cmx[:, 127:128], ident[0:64, 0:64], True, True, ["cm", "ident"], [("bank", 2)])
    rw = [p.sbuf([1, 64], F32) for _ in range(2)]
    p.copy("dve", rw[0][:, :], bank[2][0:1, 0:64], [("bank", 2)], ["rw"])
    rmax = _scan_free(p, "dve", rw, 64, ALU.max, "rw")
    rsh = p.sbuf([1, 64], F32)
    p.memset("dve", rsh[:, :], 0.0, ["rsh"])
    p.copy("dve", rsh[:, 1:64], rmax[:, 0:63], ["rw", "rsh"], ["rsh"])
    p.mm(bank[2][0:64, 64:65], rsh[0:1, :], ones_f[0:1, 0:1], True, True, ["rsh", "ones_f"], [("bank", 2)])
    pcol = p.sbuf([64, 1], F32)
    p.copy("dve", pcol[:, :], bank[2][0:64, 64:65], [("bank", 2)], ["pcol"])
    A_G = p.sbuf([64, 128], F32)
    p.ts("dve", A_G[:, :], cmx[:, :], pcol[:, 0:1], None, ALU.max, None, ["cm", "pcol"], ["A_G"])
    lb_G = p.sbuf([64, 128], F32)
    p.tt("dve", lb_G[:, :], nF_G[:, :], A_G[:, :], ALU.subtract, ["nF_G", "A_G"], ["lb_G"])
    p.act(lb_G[:, :], lb_G[:, :], AF.Exp, ["lb_G"], ["lb_G"])
    arep = p.sbuf([64, 128], F32)
    p.memset("dve", arep[:, :], 0.0, ["arep"])
    p.ts("dve", arep[:, :], arep[:, :], A_G[:, 127:128], None, ALU.add, None, ["arep", "A_G"], ["arep"])
    p.mm(bank[3][:, 0:64], arep[:, :], ident[0:64, 0:64], True, True, ["arep", "ident"], [("bank", 3)])
    aend = p.sbuf([128, 64], F32)
    aendp = p.sbuf([128, 64], F32)
    p.copy("dve", aend[:, :], bank[3][:, 0:64], [("bank", 3)], ["aend"])
    p.memset("dve", aendp[:, :], 0.0, ["aendp"])
    p.copy("dve", aendp[:, 1:64], aend[:, 0:63], ["aend", "aendp"], ["aendp"])
    w_tm = p.sbuf([128, 64], F32)
    p.tt("dve", w_tm[:, :], aend[:, :], a_tm[:, :], ALU.subtract, ["aend", "a_tm"], ["w_tm"])
    p.ts("dve", w_tm[:, :], w_tm[:, :], 0.0, None, ALU.max, None, ["w_tm"], ["w_tm"])
    p.act(w_tm[:, :], w_tm[:, :], AF.Exp, ["w_tm"], ["w_tm"], scale=-1.0)
    dec = p.sbuf([128, 64], F32)
    p.tt("dve", dec[:, :], aend[:, :], aendp[:, :], ALU.subtract, ["aend", "aendp"], ["dec"])
    p.act(dec[:, :], dec[:, :], AF.Exp, ["dec"], ["dec"], scale=-1.0)

    qT = p.sbuf([128, S], BF16)
    kT = p.sbuf([128, S], BF16)
    CH = 2048
    cbuf = [p.sbuf([128, CH + 4], F32) for _ in range(2)]
    cacc = [p.sbuf([128, CH], F32) for _ in range(2)]
    it = 0
    for which, src, dst, c0 in (("q", d["mqT"], qT, 0), ("k", d["mkT"], kT, 5)):
        for g in range(S // CH):
            r = it % 2
            it += 1
            lo = g * CH - 2
            hi = (g + 1) * CH + 2
            if lo < 0:
                p.memset("pool", cbuf[r][:, 0:2], 0.0, [("cbuf", r)])
            if hi > S:
                p.memset("pool", cbuf[r][:, CH + 2:CH + 4], 0.0, [("cbuf", r)])
            slo, shi = max(lo, 0), min(hi, S)
            p.dma("sp", cbuf[r][:, slo - lo:shi - lo], src[:, slo:shi], f"ld_cb{r}", writes=[("cbuf", r)])
            p.ts("dve", cacc[r][:, :], cbuf[r][:, 0:CH], mpar[:, c0:c0 + 1], None, ALU.mult, None, [("cbuf", r), "mpar"], [("cacc", r)])
            for j in range(1, 5):
                p.stt("dve", cacc[r][:, :], cbuf[r][:, j:j + CH], mpar[:, c0 + j:c0 + j + 1], cacc[r][:, :], ALU.mult, ALU.add,
                      [("cbuf", r), ("cacc", r), "mpar"], [("cacc", r)])
            p.act(dst[:, g * CH:(g + 1) * CH], cacc[r][:, :], AF.Silu, [("cacc", r)], [(which + "T", g)])

    Cst = [p.sbuf([128, 384], F32) for _ in range(2)]
    Cbf = [p.sbuf([128, 384], BF16) for _ in range(2)]
    sel = [p.sbuf([64, 128], F32) for _ in range(2)]
    ones64 = p.sbuf([64, 128], F32)
    p.memset("pool", ones64[:, :], 1.0, ["ones64"])
    Xb = [p.sbuf([128, 128], F32) for _ in range(2)]
    Eb = [p.sbuf([128, 128], F32) for _ in range(2)]
    Yb = [p.sbuf([128, 128], F32) for _ in range(2)]
    ST = [p.sbuf([128, 128], BF16) for _ in range(2)]
    qp = [p.sbuf([128, 128], BF16) for _ in range(2)]
    kw = [p.sbuf([128, 128], BF16) for _ in range(2)]
    dab = [p.sbuf([128, 128], F32) for _ in range(2)]
    hout = [p.sbuf([128, 2, 128], F32) for _ in range(2)]
    for c in range(NT):
        r = c % 2
        tsl = slice(c * 128, (c + 1) * 128)
        g4 = c // 16
        AB = bank[r]
        p.ts("pool", sel[r][:, :], ones64[:, :], ident[0:64, c:c + 1], None, ALU.mult, None, ["ones64", "ident"], [("sel", r)])
        p.mm(AB[:, 0:128], sel[r][:, :], A_G[:, :], True, True, [("sel", r), "A_G"], [("bank", r)])
        p.mm(AB[:, 128:256], sel[r][:, :], lb_G[:, :], True, True, [("sel", r), "lb_G"], [("bank", r)])
        p.mm(AB[:, 256:384], kT[:, tsl], qT[:, tsl], True, True, [("kT", g4), ("qT", g4)], [("bank", r)])
        p.ts("dve", Xb[r][:, :], AB[:, 0:128], a_tm[:, c:c + 1], 0.0, ALU.subtract, ALU.max, [("bank", r), "a_tm"], [("X", r)])
        p.tt("pool", Xb[r][:, :], Xb[r][:, :], maskp[:, :], ALU.add, [("X", r), "maskp"], [("X", r)])
        p.act(Eb[r][:, :], Xb[r][:, :], AF.Exp, [("X", r)], [("E", r)], scale=-1.0, bias=LNS)
        p.tt("dve", ST[r][:, :], AB[:, 256:384], Eb[r][:, :], ALU.mult, [("bank", r), ("E", r)], [("ST", r)])
        p.ts("dve", Yb[r][:, :], AB[:, 0:128], aendp[:, c:c + 1], None, ALU.subtract, None, [("bank", r), "aendp"], [("Y", r)])
        p.act(Yb[r][:, :], Yb[r][:, :], AF.Exp, [("Y", r)], [("Y", r)], scale=-1.0, bias=LNS)
        p.tt("pool", qp[r][:, :], qT[:, tsl], Yb[r][:, :], ALU.mult, [("qT", g4), ("Y", r)], [("qp", r)])
        p.op("pe", lambda e, r=r, tsl=tsl: e.transpose(psT[r][:, :], kT[:, tsl], ident_bf[:, :]), [("kT", g4), "ident_bf"], [("psT", r)])
        p.act(kw[r][:, :], psT[r][:, :], AF.Copy, [("psT", r), "w_tm"], [("kw", r)], scale=w_tm[:, c:c + 1])
        U = bank[2 + r]
        p.mm(U[:, 0:256], kw[r][:, :], v_bf[:, c, :], True, True, [("kw", r), ("mv", g4)], [("bank", 2 + r)])
        p.mm(U[:, 256:384], kw[r][:, :], ones_bf[:, :], True, True, [("kw", r), "ones_bf"], [("bank", 2 + r)])
        H = bank[4 + r]
        prev = (c - 1) % 2
        for hh in range(3):
            cs = slice(hh * 128, (hh + 1) * 128)
            lhs2 = v_bf[:, c, cs] if hh < 2 else ones_bf[:, :]
            if c > 0:
                p.mm(H[:, cs], Cbf[prev][:, cs], qp[r][:, :], True, False, [("Cbf", prev), ("qp", r)], [("bank", 4 + r)])
            p.mm(H[:, cs], lhs2, ST[r][:, :], c == 0, True, [("mv", g4), "ones_bf", ("ST", r)], [("bank", 4 + r)])
        if c == 0:
            p.copy("dve", Cst[r][:, :], U[:, 0:384], [("bank", 2 + r)], [("Cst", r)])
        else:
            p.stt("dve", Cst[r][:, :], Cst[prev][:, :], dec[:, c:c + 1], U[:, 0:384], ALU.mult, ALU.add,
                  [("Cst", prev), "dec", ("bank", 2 + r)], [("Cst", r)])
        p.copy("act", Cbf[r][:, :], Cst[r][:, :], [("Cst", r)], [("Cbf", r)])
        p.act(dab[r][:, :], H[:, 256:384], AF.Abs, [("bank", 4 + r)], [("dab", r)])
        p.tt("dve", dab[r][:, :], dab[r][:, :], AB[:, 128:256], ALU.max, [("dab", r), ("bank", r)], [("dab", r)])
        p.op("dve", lambda e, r=r: e.reciprocal(out=dab[r][:, :], in_=dab[r][:, :]), [("dab", r)], [("dab", r)])
        for hh in range(2):
            p.tt("dve", hout[r][:, hh, :], H[:, hh * 128:(hh + 1) * 128], dab[r][:, :], ALU.mult,
                 [("bank", 4 + r), ("dab", r)], [("hout", r, hh)])
        p.dma("sp", d["hmT"].rearrange("(h p) t -> p h t", p=128)[:, :, tsl], hout[r][:, :, :], f"st_h{r}",
              reads=[("hout", r, 0), ("hout", r, 1)], is_out=True)


def prep_B_mlstm(zf, zv, conv_w_l, gate_b_l):
    tri, maskp, ident = mlstm_consts()
    maps = []
    for j in range(NCORES):
        h, dr = j % 4, j // 4
        mqT = np.concatenate([zf[i][h * 128:(h + 1) * 128] for i in range(NCORES)], axis=1)
        mkT = np.concatenate([zf[i][512 + h * 128:512 + (h + 1) * 128] for i in range(NCORES)], axis=1)
        mv = np.concatenate([zv[i][:, h * 256:(h + 1) * 256] for i in range(NCORES)], axis=0)
        gi = np.concatenate([zf[i][3072 + (2 * dr) * 4 + h] for i in range(NCORES)], axis=0)
        gf = np.concatenate([zf[i][3072 + (2 * dr + 1) * 4 + h] for i in range(NCORES)], axis=0)
        cq = conv_w_l[:, h * 128:(h + 1) * 128]
        ck = conv_w_l[:, 512 + h * 128:512 + (h + 1) * 128]
        if dr == 1:
            mqT, mkT, mv, gi, gf = mqT[:, ::-1], mkT[:, ::-1], mv[::-1], gi[::-1], gf[::-1]
            cq, ck = cq[::-1], ck[::-1]
        gtm = np.concatenate([gi.reshape(64, 128).T, gf.reshape(64, 128).T], axis=1)
        mpar = np.concatenate([cq.T, ck.T, np.full((128, 1), gate_b_l[2 * dr, h], np.float32),
                               np.full((128, 1), gate_b_l[2 * dr + 1, h], np.float32)], axis=1)
        maps.append({"mqT": np.ascontiguousarray(mqT), "mkT": np.ascontiguousarray(mkT), "mv": np.ascontiguousarray(mv),
                     "gtm": np.ascontiguousarray(gtm, dtype=np.float32), "mpar": np.ascontiguousarray(mpar, dtype=np.float32),
                     "tri": tri, "maskp": maskp, "ident": ident})
    return maps


NR = 36


def build_C():
    p = Prog()
    T = TPC
    xT_d = p.dram("xT", [D, T], F32, "ExternalInput")
    par_d = p.dram("par", [128, 8 * KC], F32, "ExternalInput")
    wmo_d = p.dram("wmo", [8, 128, KC, 128], F32, "ExternalInput")
    wgm_d = p.dram("wgm", [16, 128, KC, 128], F32, "ExternalInput")
    wga_d = p.dram("wga", [16, 128, KC, 128], F32, "ExternalInput")
    wbm_d = p.dram("wbm", [16, 128, 8, 128], F32, "ExternalInput")
    wba_d = p.dram("wba", [16, 128, 8, 128], F32, "ExternalInput")
    wo_d = p.dram("wo", [16, 128, KC, 128], F32, "ExternalInput")
    hf_d = p.dram("hfT", [1024, T], F32, "ExternalInput")
    hb_d = p.dram("hbT", [1024, T], F32, "ExternalInput")
    ya_d = p.dram("yaT", [1024, T], F32, "ExternalInput")
    rw_d = p.dram("rw", [128, KC, NR], F32, "ExternalInput")
    rb_d = p.dram("rb", [128, 1], F32, "ExternalInput")
    ident_d = p.dram("ident", [128, 128], F32, "ExternalInput")
    iota_d = p.dram("iota", [128, 12], F32, "ExternalInput")
    xo_d = p.dram("xo", [D, T], F32, "ExternalOutput")
    h2_d = p.dram("h2T", [D, T], BF16, "ExternalOutput")
    rt_d = p.dram("rout", [T, 4], F32, "ExternalOutput")
    X = mybir.AxisListType.X

    par = p.sbuf([128, 8 * KC], F32)
    ones_f = p.sbuf([128, 128], F32)
    rw = p.sbuf([128, KC, NR], F32)
    rb = p.sbuf([128, 1], F32)
    ident = p.sbuf([128, 128], F32)
    iota = p.sbuf([128, 12], F32)
    merged = p.sbuf([128, KC, T], BF16)
    p.memset("pool", ones_f[:, :], 1.0, ["ones_f"])
    p.dma("sp", par[:, :], par_d[:, :], "ld_par", writes=["par"])
    p.dma("sp", rw[:, :, :], rw_d[:, :, :], "ld_rw", writes=["rw"])
    p.dma("sp", rb[:, :], rb_d[:, :], "ld_rb", writes=["rb"])
    p.dma("sp", ident[:, :], ident_d[:, :], "ld_ident", writes=["ident"])
    p.dma("sp", iota[:, :], iota_d[:, :], "ld_iota", writes=["iota"])
    ps_ss = [p.psum([128, 512]) for _ in range(2)]
    ps = [p.psum([128, 512]) for _ in range(6)]
    xv = xT_d.rearrange("(k p) t -> p k t", p=128)
    xr = [p.sbuf([128, T], F32) for _ in range(3)]
    xcnt = [0]

    def getx_stream(k, pass_no):
        r = xcnt[0] % 3
        xcnt[0] += 1
        p.dma("sp", xr[r][:, :], xv[:, k, :], f"ld_xr{r}", writes=[("xr", r)])
        return xr[r][:, :], ("xr", r)

    wring = [p.sbuf([128, KC, 128], BF16) for _ in range(4)]
    wcnt = [0]

    def loadw(src, nk=KC):
        r = wcnt[0] % 4
        wcnt[0] += 1
        p.dma("pool", wring[r][:, 0:nk, :], src, f"ld_w{r}", writes=[("w", r)])
        return wring[r], ("w", r)

    with p.scope():
        hT = p.sbuf([128, KC, T], BF16)
        ymT = p.sbuf([128, 8, T], BF16)
        yaT = p.sbuf([128, 8, T], BF16)
        _norm_mod(p, getx_stream, hT, par[:, 0:KC], par[:, KC:2 * KC], par[:, 2 * KC:3 * KC], ones_f, ps_ss, "par")
        hkeys = [("parh", k) for k in range(KC)]
        for g in range(2):
            p.dma("pool", yaT[:, 4 * g:4 * g + 4, :], ya_d.rearrange("(k p) t -> p k t", p=128)[:, 4 * g:4 * g + 4, :], "ld_ya",
                  writes=[("ya", g)])
        hs = [p.sbuf([128, 2, T], F32) for _ in range(2)]
        hb2 = [p.sbuf([128, 2, T], F32)] * 2
        hq = p.sbuf([128, T], F32)
        hr = p.sbuf([128, T], F32)
        sg = [p.sbuf([128, T], F32) for _ in range(2)]
        mi = 0
        for hd in range(4):
            r = hd % 2
            rows = slice(hd * 256, (hd + 1) * 256)
            p.dma("sp", hs[r][:, :, :], hf_d[rows, :].rearrange("(k p) t -> p k t", p=128), f"ld_hs{r}", writes=[("hs", r)])
            p.dma("sp", hb2[r][:, :, :], hb_d[rows, :].rearrange("(k p) t -> p k t", p=128), "ld_hb", writes=[("hb", 0)])
            p.tt("pool", hs[r][:, :, :], hs[r][:, :, :], hb2[r][:, :, :], ALU.add, [("hs", r), ("hb", 0)], [("hs", r)])
            for c2 in range(2):
                p.act(hq[:, :], hs[r][:, c2, :], AF.Square, [("hs", r)], ["hq"])
                for h in range(2):
                    p.mm(ps_ss[h][:, :], ones_f[:, :], hq[:, h * 512:(h + 1) * 512], c2 == 0, c2 == 1, ["ones_f", "hq"], [("parss", h)])
            for h in range(2):
                p.act(hr[:, h * 512:(h + 1) * 512], ps_ss[h][:, :], AF.Sqrt, [("parss", h)], ["hr"], bias=EPS, scale=1.0 / 256)
            p.op("dve", lambda e: e.reciprocal(out=hr[:, :], in_=hr[:, :]), ["hr"], ["hr"])
            for c2 in range(2):
                kb = hd * 2 + c2
                w, wk = loadw(wmo_d[kb])
                s_ = sg[kb % 2]
                for h in range(2):
                    pb = ps[mi % 6]
                    pk = ("ps", mi % 6)
                    mi += 1
                    for k in range(KC):
                        p.mm(pb[:, :], w[:, k, :], hT[:, k, h * 512:(h + 1) * 512], k == 0, k == KC - 1, [wk, hkeys[k]], [pk])
                    p.act(s_[:, h * 512:(h + 1) * 512], pb[:, :], AF.Sigmoid, [pk], [("sg", kb % 2)])
                p.stt("dve", hs[r][:, c2, :], hs[r][:, c2, :], par[:, 7 * KC + kb:7 * KC + kb + 1], hr[:, :], ALU.mult, ALU.mult,
                      [("hs", r), "hr", "par"], [("hs", r)])
                p.tt("dve", ymT[:, kb, :], hs[r][:, c2, :], s_[:, :], ALU.mult, [("hs", r), ("sg", kb % 2)], [("ym", kb)])
        t1 = [p.sbuf([128, 512], F32) for _ in range(2)]
        t2 = [p.sbuf([128, 512], F32) for _ in range(2)]
        s1 = [p.sbuf([128, 512], F32) for _ in range(2)]
        s2 = [p.sbuf([128, 512], F32) for _ in range(2)]
        it = 0
        for cb in range(16):
            wgm, kgm = loadw(wgm_d[cb])
            wga, kga = loadw(wga_d[cb])
            wbm, kbm = loadw(wbm_d[cb], 8)
            wba, kba = loadw(wba_d[cb], 8)
            for h in range(2):
                hsl = slice(h * 512, (h + 1) * 512)
                r = it % 2
                it += 1
                banks = []
                for w, wk, src, nk, skeys in ((wgm, kgm, hT, KC, hkeys), (wga, kga, hT, KC, hkeys),
                                              (wbm, kbm, ymT, 8, [("ym", k) for k in range(8)]),
                                              (wba, kba, yaT, 8, [("ya", k // 4) for k in range(8)])):
                    pb = ps[mi % 6]
                    pk = ("ps", mi % 6)
                    mi += 1
                    for k in range(nk):
                        p.mm(pb[:, :], w[:, k, :], src[:, k, hsl], k == 0, k == nk - 1, [wk, skeys[k]], [pk])
                    banks.append((pb, pk))
                p.act(s1[r][:, :], banks[0][0][:, :], AF.Sigmoid, [banks[0][1]], [("s1", r)])
                p.act(s2[r][:, :], banks[1][0][:, :], AF.Sigmoid, [banks[1][1]], [("s2", r)])
                p.tt("dve", t1[r][:, :], banks[2][0][:, :], s1[r][:, :], ALU.mult, [banks[2][1], ("s1", r)], [("t1", r)])
                p.tt("dve", t2[r][:, :], banks[3][0][:, :], s2[r][:, :], ALU.mult, [banks[3][1], ("s2", r)], [("t2", r)])
                p.tt("pool", merged[:, cb, hsl], t1[r][:, :], t2[r][:, :], ALU.add, [("t1", r), ("t2", r)], [("mg", cb)])

    with p.scope():
        xn = p.sbuf([128, KC, T], F32)
        h2T = p.sbuf([128, KC, T], BF16)
        mi = 0
        for cb in range(16):
            w, wk = loadw(wo_d[cb])
            r = xcnt[0] % 3
            xcnt[0] += 1
            p.dma("sp", xr[r][:, :], xv[:, cb, :], f"ld_xr{r}", writes=[("xr", r)])
            for h in range(2):
                hsl = slice(h * 512, (h + 1) * 512)
                pb = ps[mi % 4]
                pk = ("ps", mi % 4)
                mi += 1
                for k in range(KC):
                    p.mm(pb[:, :], w[:, k, :], merged[:, k, hsl], k == 0, k == KC - 1, [wk, ("mg", k)], [pk])
                p.stt("dve", xn[:, cb, hsl], pb[:, :], par[:, 3 * KC + cb:3 * KC + cb + 1], xr[r][:, hsl], ALU.mult, ALU.add,
                      [pk, "par", ("xr", r)], [("xn", cb, h)])
        for g in range(4):
            p.dma("sp", xo_d.rearrange("(k p) t -> p k t", p=128)[:, 4 * g:4 * g + 4, :], xn[:, 4 * g:4 * g + 4, :], f"st_x{g}",
                  reads=[("xn", k, h) for k in range(4 * g, 4 * g + 4) for h in range(2)], is_out=True)
        psr = ps[0]
        LT = p.sbuf([NR, T], F32)

        def router_hook(k, f, fkey):
            for h in range(2):
                p.mm(ps[4 + h][0:NR, :], rw[:, k, :], f[:, h * 512:(h + 1) * 512], k == 0, k == KC - 1, ["rw", fkey], [("ps", 4 + h)])

        class _XN:
            pass

        def getx_res(k, pass_no):
            return xn[:, k, :], ("xnall", k)

        for k in range(KC):
            p.op("pool", lambda e: e.engine_nop(), [("xn", k, 0), ("xn", k, 1)], [("xnall", k)])
        _norm_mod(p, getx_res, h2T, par[:, 4 * KC:5 * KC], par[:, 5 * KC:6 * KC], par[:, 6 * KC:7 * KC], ones_f, ps_ss, "par2",
                  hook=router_hook, tagpar="par")
        for g in range(4):
            p.dma("sp", h2_d.rearrange("(k p) t -> p k t", p=128)[:, 4 * g:4 * g + 4, :], h2T[:, 4 * g:4 * g + 4, :], f"st_h2{g}",
                  reads=[("par2h", k) for k in range(4 * g, 4 * g + 4)], is_out=True)
        NTT = T // 128
        for h in range(2):
            p.act(LT[:, h * 512:(h + 1) * 512], ps[4 + h][0:NR, :], AF.Identity, [("ps", 4 + h), "rb"], [("LT", h)], bias=rb[0:NR, 0:1])
        for tt_ in range(NTT):
            p.mm(psr[:, tt_ * NR:(tt_ + 1) * NR], LT[:, tt_ * 128:(tt_ + 1) * 128], ident[0:NR, 0:NR], True, True,
                 [("LT", tt_ // 4), "ident"], [("ps", 0)])
        L = p.sbuf([128, NTT, NR], F32)
        rout = p.sbuf([128, NTT, 4], F32)
        sc = p.sbuf([128, NTT, 16], F32)
        ohg = p.sbuf([128, NTT, 4], F32)
        ge = p.sbuf([128, NTT, 4], F32)
        wi = p.sbuf([128, NTT, 8], F32)
        wi2 = p.sbuf([128, NTT, 8], F32)
        oh1 = p.sbuf([128, NTT, 8], F32)
        oh2 = p.sbuf([128, NTT, 8], F32)
        tm8 = p.sbuf([128, NTT, 8], F32)
        for tt_ in range(NTT):
            K_ = ("rt", tt_)
            Lt = L[:, tt_, :]
            s = lambda j: sc[:, tt_, j:j + 1]
            p.copy("dve", Lt, psr[:, tt_ * NR:(tt_ + 1) * NR], [("ps", 0)], [K_])
            R_ = lambda fn: p.op("dve", fn, [K_, "iota"], [K_])
            R_(lambda e, Lt=Lt, tt_=tt_: e.reduce_max(out=sc[:, tt_, 0:1], in_=Lt[:, 0:4], axis=X))
            R_(lambda e, Lt=Lt, tt_=tt_: e.tensor_scalar(out=ge[:, tt_, :], in0=Lt[:, 0:4], scalar1=sc[:, tt_, 0:1], scalar2=None, op0=ALU.subtract))
            p.act(ge[:, tt_, :], ge[:, tt_, :], AF.Exp, [K_], [K_])
            R_(lambda e, tt_=tt_: e.reduce_sum(out=sc[:, tt_, 1:2], in_=ge[:, tt_, :], axis=X))
            R_(lambda e, tt_=tt_: e.reciprocal(out=sc[:, tt_, 2:3], in_=sc[:, tt_, 1:2]))
            R_(lambda e, Lt=Lt, tt_=tt_: e.tensor_scalar(out=ohg[:, tt_, :], in0=Lt[:, 0:4], scalar1=sc[:, tt_, 0:1], scalar2=None, op0=ALU.is_ge))
            R_(lambda e, Lt=Lt, tt_=tt_: e.tensor_scalar(out=wi[:, tt_, :], in0=Lt[:, 4:12], scalar1=ohg[:, tt_, 0:1], scalar2=None, op0=ALU.mult))
            for g in range(1, 4):
                R_(lambda e, Lt=Lt, tt_=tt_, g=g: e.scalar_tensor_tensor(out=wi[:, tt_, :], in0=Lt[:, 4 + 8 * g:12 + 8 * g], scalar=ohg[:, tt_, g:g + 1],
                                                                        in1=wi[:, tt_, :], op0=ALU.mult, op1=ALU.add))
            R_(lambda e, tt_=tt_: e.reduce_max(out=sc[:, tt_, 3:4], in_=wi[:, tt_, :], axis=X))
            R_(lambda e, tt_=tt_: e.tensor_scalar(out=oh1[:, tt_, :], in0=wi[:, tt_, :], scalar1=sc[:, tt_, 3:4], scalar2=None, op0=ALU.is_ge))
            R_(lambda e, tt_=tt_: e.scalar_tensor_tensor(out=wi2[:, tt_, :], in0=oh1[:, tt_, :], scalar=-1e30, in1=wi[:, tt_, :], op0=ALU.mult, op1=ALU.add))
            R_(lambda e, tt_=tt_: e.reduce_max(out=sc[:, tt_, 4:5], in_=wi2[:, tt_, :], axis=X))
            R_(lambda e, tt_=tt_: e.tensor_scalar(out=oh2[:, tt_, :], in0=wi2[:, tt_, :], scalar1=sc[:, tt_, 4:5], scalar2=None, op0=ALU.is_ge))
            R_(lambda e, tt_=tt_: e.tensor_tensor(out=sc[:, tt_, 5:6], in0=sc[:, tt_, 4:5], in1=sc[:, tt_, 3:4], op=ALU.subtract))
            p.act(sc[:, tt_, 6:7], sc[:, tt_, 5:6], AF.Exp, [K_], [K_])
            R_(lambda e, tt_=tt_: e.tensor_scalar(out=sc[:, tt_, 7:8], in0=sc[:, tt_, 6:7], scalar1=1.0, scalar2=None, op0=ALU.add))
            R_(lambda e, tt_=tt_: e.reciprocal(out=sc[:, tt_, 7:8], in_=sc[:, tt_, 7:8]))
            R_(lambda e, tt_=tt_: e.tensor_tensor(out=rout[:, tt_, 2:3], in0=sc[:, tt_, 7:8], in1=sc[:, tt_, 2:3], op=ALU.mult))
            R_(lambda e, tt_=tt_: e.tensor_tensor(out=rout[:, tt_, 3:4], in0=rout[:, tt_, 2:3], in1=sc[:, tt_, 6:7], op=ALU.mult))
            R_(lambda e, tt_=tt_: e.tensor_tensor(out=ge[:, tt_, :], in0=ohg[:, tt_, :], in1=iota[:, 8:12], op=ALU.mult))
            R_(lambda e, tt_=tt_: e.reduce_sum(out=sc[:, tt_, 8:9], in_=ge[:, tt_, :], axis=X))
            R_(lambda e, tt_=tt_: e.tensor_tensor(out=tm8[:, tt_, :], in0=oh1[:, tt_, :], in1=iota[:, 0:8], op=ALU.mult))
            R_(lambda e, tt_=tt_: e.reduce_sum(out=sc[:, tt_, 9:10], in_=tm8[:, tt_, :], axis=X))
            R_(lambda e, tt_=tt_: e.tensor_tensor(out=tm8[:, tt_, :], in0=oh2[:, tt_, :], in1=iota[:, 0:8], op=ALU.mult))
            R_(lambda e, tt_=tt_: e.reduce_sum(out=sc[:, tt_, 10:11], in_=tm8[:, tt_, :], axis=X))
            R_(lambda e, tt_=tt_: e.scalar_tensor_tensor(out=rout[:, tt_, 0:1], in0=sc[:, tt_, 8:9], scalar=8.0, in1=sc[:, tt_, 9:10], op0=ALU.mult, op1=ALU.add))
            R_(lambda e, tt_=tt_: e.scalar_tensor_tensor(out=rout[:, tt_, 1:2], in0=sc[:, tt_, 8:9], scalar=8.0, in1=sc[:, tt_, 10:11], op0=ALU.mult, op1=ALU.add))
        p.dma("sp", rt_d.rearrange("(t p) c -> p t c", p=128), rout[:, :, :], "st_rt", reads=[("rt", t) for t in range(NTT)], is_out=True)
    return p


def _blkN(w, c0, nblk, kc=KC):
    sub = w[:, c0:c0 + nblk * 128].reshape(kc, 128, nblk, 128)
    return np.ascontiguousarray(sub.transpose(2, 1, 0, 3))


def prep_C_weights(lw):
    w_in = lw["w_in"]
    out = {
        "wmo": _blkN(w_in, C_MO, 8), "wgm": _blkN(w_in, C_GM, 16), "wga": _blkN(w_in, C_GA, 16),
        "wbm": _blkN(lw["w_branch_m"], 0, 16, 8), "wba": _blkN(lw["w_branch_a"], 0, 16, 8), "wo": _blkN(lw["w_out"], 0, 16),
    }
    rwf = np.concatenate([lw["rg_w"], lw["re_w"]], axis=1)
    out["rw"] = np.ascontiguousarray(rwf.reshape(KC, 128, NR).transpose(1, 0, 2))
    rbc = np.zeros((128, 1), np.float32)
    rbc[:NR, 0] = np.concatenate([lw["rg_b"], lw["re_b"]])
    out["rb"] = rbc
    out["ident"] = np.eye(128, dtype=np.float32)
    out["iota"] = np.ascontiguousarray(np.tile(np.concatenate([np.arange(8), np.arange(4)])[None, :], (128, 1)).astype(np.float32))
    return out


def run_C(xT_sh, mod_l, lw, hmT, yaT):
    sh1, sc1, g1, sh2, sc2, g2 = [mod_l[i * D:(i + 1) * D] for i in range(6)]
    par = np.concatenate([_pk(lw["norm1_g"]), _pk(sc1), _pk(sh1), _pk(g1), _pk(lw["norm2_g"]), _pk(sc2), _pk(sh2),
                          _pk(lw["m_norm_g"]), np.zeros((128, 8), np.float32)], axis=1)
    cw = prep_C_weights(lw)
    in_maps = []
    for i in range(NCORES):
        ts = slice(i * TPC, (i + 1) * TPC)
        m = dict(cw)
        m["xT"] = xT_sh[i]
        m["par"] = par
        m["hfT"] = np.ascontiguousarray(np.concatenate([hmT[h][:, ts] for h in range(4)], axis=0))
        m["hbT"] = np.ascontiguousarray(np.concatenate([hmT[4 + h][:, ts] for h in range(4)], axis=0))
        m["yaT"] = np.ascontiguousarray(np.concatenate([yaT[j][:, ts] for j in range(8)], axis=0))
        in_maps.append(m)
    res = _run(build_C(), in_maps)
    return [r["xo"] for r in res], [r["h2T"] for r in res], [r["rout"] for r in res]


EPC = 4
DE = 512


def build_D(NJ, cap):
    p = Prog()
    hT_d = p.dram("hT", [NJ, D, cap], BF16, "ExternalInput")
    cw_d = p.dram("cw", [NJ, 128, cap // 128], F32, "ExternalInput")
    wg_d = p.dram("wg", [NJ, 128, KC, DE], F32, "ExternalInput")
    wu_d = p.dram("wu", [NJ, 128, KC, DE], F32, "ExternalInput")
    wd_d = p.dram("wd", [NJ, 128, 4, D], F32, "ExternalInput")
    y_d = p.dram("y", [NJ, cap, D], F32, "ExternalOutput")
    hsb = [p.sbuf([128, KC, cap], BF16) for _ in range(2)]
    cws = [p.sbuf([128, cap // 128], F32) for _ in range(2)]
    wg = [p.sbuf([128, KC, DE], BF16) for _ in range(2)]
    wu = [p.sbuf([128, KC, DE], BF16) for _ in range(2)]
    wd = [p.sbuf([128, 4, D], BF16) for _ in range(2)]
    aT = p.sbuf([128, 4, cap], BF16)
    sgb = [p.sbuf([128, 512], F32) for _ in range(2)]
    yst = [p.sbuf([128, D], F32) for _ in range(2)]
    ps = [p.psum([128, 512]) for _ in range(8)]
    mi = 0
    si = 0
    yi = 0
    for j in range(NJ):
        r = j % 2
        we = j % 2
        for g in range(4):
            p.dma("sp", hsb[r][:, 4 * g:4 * g + 4, :], hT_d[j].rearrange("(k p) t -> p k t", p=128)[:, 4 * g:4 * g + 4, :], f"ld_h{r}",
                  writes=[("h", r, g)])
        p.dma("sp", cws[r][:, :], cw_d[j], f"ld_cw{r}", writes=[("cw", r)])
        for g in range(4):
            p.dma("pool", wg[we][:, 4 * g:4 * g + 4, :], wg_d[j][:, 4 * g:4 * g + 4, :], f"ld_wg{we}", writes=[("wg", we, g)])
            p.dma("pool", wu[we][:, 4 * g:4 * g + 4, :], wu_d[j][:, 4 * g:4 * g + 4, :], f"ld_wu{we}", writes=[("wu", we, g)])
        for g in range(4):
            p.dma("pool", wd[we][:, g, :], wd_d[j][:, g, :], f"ld_wd{we}", writes=[("wd", we, g)])
        nb = (cap + 511) // 512
        for tb in range(nb):
            n0 = tb * 512
            n = min(512, cap - n0)
            for fb in range(4):
                pg = ps[mi % 8]
                kg = ("ps", mi % 8)
                mi += 1
                pu = ps[mi % 8]
                ku = ("ps", mi % 8)
                mi += 1
                for k in range(KC):
                    p.mm(pg[:, 0:n], wg[we][:, k, fb * 128:(fb + 1) * 128], hsb[r][:, k, n0:n0 + n], k == 0, k == KC - 1,
                         [("wg", we, k // 4), ("h", r, k // 4)], [kg])
                for k in range(KC):
                    p.mm(pu[:, 0:n], wu[we][:, k, fb * 128:(fb + 1) * 128], hsb[r][:, k, n0:n0 + n], k == 0, k == KC - 1,
                         [("wu", we, k // 4), ("h", r, k // 4)], [ku])
                s_ = sgb[si % 2]
                sk = ("sg", si % 2)
                si += 1
                p.act(s_[:, 0:n], pg[:, 0:n], AF.Silu, [kg], [sk])
                p.tt("dve", aT[:, fb, n0:n0 + n], pu[:, 0:n], s_[:, 0:n], ALU.mult, [ku, sk], [("aT", tb)])
        for tt_ in range(cap // 128):
            ys = yst[yi % 2]
            yk = ("yst", yi % 2)
            yi += 1
            for cb in range(4):
                py = ps[mi % 8]
                ky = ("ps", mi % 8)
                mi += 1
                for fb in range(4):
                    p.mm(py[:, :], aT[:, fb, tt_ * 128:(tt_ + 1) * 128], wd[we][:, fb, cb * 512:(cb + 1) * 512], fb == 0, fb == 3,
                         [("aT", tt_ // 4), ("wd", we, fb)], [ky])
                if cb % 2 == 0:
                    p.act(ys[:, cb * 512:(cb + 1) * 512], py[:, :], AF.Copy, [ky, ("cw", r)], [(yk, cb)], scale=cws[r][:, tt_:tt_ + 1])
                else:
                    p.ts("dve", ys[:, cb * 512:(cb + 1) * 512], py[:, :], cws[r][:, tt_:tt_ + 1], None, ALU.mult, None, [ky, ("cw", r)], [(yk, cb)])
            p.dma("sp", y_d[j][tt_ * 128:(tt_ + 1) * 128, :], ys[:, :], f"st_y{yi % 2}", reads=[(yk, cb) for cb in range(4)], is_out=True)
    return p


def _plan_D(counts):
    best = None
    for cap in (384, 512, 640, 768, 1024):
        nj = -(-int(sum(-(-int(c) // cap) for c in counts if c > 0)) // NCORES)
        nj = max(nj, 1)
        cost = nj * (70.0 + 0.08 * cap)
        if best is None or cost < best[0]:
            best = (cost, cap, nj)
    return best[1], best[2]


def run_D(h2T_sh, rout_sh, lw):
    h2 = np.concatenate([np.asarray(h).view(np.uint16).T for h in h2T_sh], axis=0)
    bf = np.asarray(h2T_sh[0]).dtype
    rout = np.concatenate(rout_sh, axis=0)
    ids = rout[:, 0:2].astype(np.int64)
    wts = rout[:, 2:4]
    toks, slots = [], []
    for e in range(32):
        t, s_ = np.nonzero(ids == e)
        toks.append(t)
        slots.append(s_)
    cap, NJ = _plan_D([len(t) for t in toks])
    jobs = []
    for e in range(32):
        for s0 in range(0, len(toks[e]), cap):
            jobs.append((e, toks[e][s0:s0 + cap], slots[e][s0:s0 + cap]))
    in_maps = []
    wgl = lw["e_w_gate"].reshape(32, KC, 128, DE)
    wul = lw["e_w_up"].reshape(32, KC, 128, DE)
    wdl = lw["e_w_down"].reshape(32, 4, 128, D)
    for i in range(NCORES):
        hT = np.zeros((NJ, D, cap), np.uint16)
        cw = np.zeros((NJ, 128, cap // 128), np.float32)
        wg = np.zeros((NJ, 128, KC, DE), np.float32)
        wu = np.zeros((NJ, 128, KC, DE), np.float32)
        wd = np.zeros((NJ, 128, 4, D), np.float32)
        for jl in range(NJ):
            jg = jl * NCORES + i
            if jg >= len(jobs):
                continue
            e, tt_, ss_ = jobs[jg]
            n = len(tt_)
            hT[jl][:, :n] = h2[tt_].T
            wfull = np.zeros(cap, np.float32)
            wfull[:n] = wts[tt_, ss_]
            cw[jl] = wfull.reshape(cap // 128, 128).T
            wg[jl] = wgl[e].transpose(1, 0, 2)
            wu[jl] = wul[e].transpose(1, 0, 2)
            wd[jl] = wdl[e].transpose(1, 0, 2)
        in_maps.append({"hT": hT.view(bf), "cw": cw, "wg": wg, "wu": wu, "wd": wd})
    res = _run(build_D(NJ, cap), in_maps)
    ys = np.zeros((2, S, D), np.float32)
    for i in range(NCORES):
        y = res[i]["y"]
        for jl in range(NJ):
            jg = jl * NCORES + i
            if jg >= len(jobs):
                continue
            e, tt_, ss_ = jobs[jg]
            ys[ss_, tt_] = y[jl][:len(tt_)]
    return [np.ascontiguousarray(ys[:, i * TPC:(i + 1) * TPC, :].transpose(0, 2, 1)) for i in range(NCORES)]


def build_E():
    p = Prog()
    T = TPC
    xT_d = p.dram("xT", [D, T], F32, "ExternalInput")
    yT_d = p.dram("yT", [2, D, T], F32, "ExternalInput")
    g_d = p.dram("g2", [128, KC], F32, "ExternalInput")
    xo_d = p.dram("xo", [D, T], F32, "ExternalOutput")
    g2 = p.sbuf([128, KC], F32)
    p.dma("sp", g2[:, :], g_d[:, :], "ld_g", writes=["g2"])
    xb = [p.sbuf([128, T], F32) for _ in range(3)]
    yb = [p.sbuf([128, 2, T], F32) for _ in range(3)]
    for k in range(KC):
        r = k % 3
        p.dma("sp", xb[r][:, :], xT_d.rearrange("(k p) t -> p k t", p=128)[:, k, :], f"ld_x{r}", writes=[("x", r)])
        p.dma("pool", yb[r][:, :, :], yT_d.rearrange("s (k p) t -> p k s t", p=128)[:, k, :, :], f"ld_y{r}", writes=[("y", r)])
        p.tt("pool", yb[r][:, 0, :], yb[r][:, 0, :], yb[r][:, 1, :], ALU.add, [("y", r)], [("y", r)])
        p.stt("dve", xb[r][:, :], yb[r][:, 0, :], g2[:, k:k + 1], xb[r][:, :], ALU.mult, ALU.add, [("y", r), ("x", r), "g2"], [("x", r)])
        p.dma("sp", xo_d.rearrange("(k p) t -> p k t", p=128)[:, k, :], xb[r][:, :], f"st_x{r}", reads=[("x", r)], is_out=True)
    return p


def run_E(xT_sh, yT_sh, g2):
    gp = _pk(g2)
    res = _run(build_E(), [{"xT": xT_sh[i], "yT": yT_sh[i], "g2": gp} for i in range(NCORES)])
    return [r["xo"] for r in res]


def run_B(zf, zv, lw, l):
    lam_init = 0.8 - 0.6 * math.exp(-0.3 * l)
    maps = prep_B_mlstm(zf, zv, lw["m_conv_w"], lw["m_gate_b"])
    cos16, sin16 = rope_tables_np()
    blk64, pm = const_mats()
    para = np.zeros((128, 8), np.float32)
    para[:, 0] = np.tile(lw["a_qnorm_g"], 2)
    para[:, 1] = np.tile(lw["a_knorm_g"], 2)
    para[:, 2] = lw["a_subln_g"]
    para[:, 3] = -lam_init
    para[:, 4] = 1.0 - lam_init
    lam_rep = np.ascontiguousarray(np.tile(lw["a_lambda"].reshape(1, 256), (128, 1)))
    for j in range(NCORES):
        m = maps[j]
        m["aqT"] = np.ascontiguousarray(np.concatenate([zf[i][1024 + j * 128:1024 + (j + 1) * 128] for i in range(NCORES)], axis=1))
        m["akT"] = np.ascontiguousarray(np.concatenate([zf[i][2048 + j * 128:2048 + (j + 1) * 128] for i in range(NCORES)], axis=1))
        m["av"] = np.ascontiguousarray(np.concatenate([zv[i][:, 1024 + j * 128:1024 + (j + 1) * 128] for i in range(NCORES)], axis=0))
        m.update({"para": para, "lam": lam_rep, "cos": cos16, "sin": sin16, "blk64": blk64, "pm": pm})
    res = _run(build_B(), maps)
    yaT = [r["yaT"] for r in res]
    hmT = [res[j]["hmT"] if j < 4 else np.ascontiguousarray(res[j]["hmT"][:, ::-1]) for j in range(NCORES)]
    return hmT, yaT


LAYER_KEYS = ["norm1_g", "norm2_g", "w_in", "m_conv_w", "m_gate_b", "m_norm_g", "a_qnorm_g", "a_knorm_g", "a_lambda", "a_subln_g",
              "w_branch_m", "w_branch_a", "w_out", "rg_w", "rg_b", "re_w", "re_b", "e_w_gate", "e_w_up", "e_w_down"]


def kernel(**inputs):
    x = np.asarray(inputs["x"], np.float32)[0]
    mod = run_mod(np.asarray(inputs["c"], np.float32), np.asarray(inputs["ada_w"], np.float32), np.asarray(inputs["ada_b"], np.float32))
    xT = [np.ascontiguousarray(x[i * TPC:(i + 1) * TPC].T) for i in range(NCORES)]
    yT = None
    g2prev = None
    for l in range(DEPTH):
        lw = {k: np.asarray(inputs[k][l], np.float32) for k in LAYER_KEYS}
        zf, zv, xT = run_A(xT, mod[l], lw["norm1_g"], lw["w_in"], g2prev, yT)
        hmT, yaT = run_B(zf, zv, lw, l)
        xT, h2T, rout = run_C(xT, mod[l], lw, hmT, yaT)
        yT = run_D(h2T, rout, lw)
        g2prev = mod[l][5 * D:6 * D]
    xo = run_E(xT, yT, g2prev)
    out = np.concatenate([o.T for o in xo], axis=0)[None]
    return np.ascontiguousarray(out.astype(np.float32))
```

```python
import contextlib
import math
import numpy as np
import concourse.bass as bass
import concourse.mybir as mybir
from concourse.bass_utils import run_bass_kernel_spmd

F32 = mybir.dt.float32
BF16 = mybir.dt.bfloat16
I32 = mybir.dt.int32
AF = mybir.ActivationFunctionType
ALU = mybir.AluOpType

NCORES = 8
D = 2048
S = 8192
TPC = S // NCORES
DEPTH = 4
KC = D // 128
EPS = 1e-6
N_IN = 10256
C_MQ, C_MK, C_MV, C_MG, C_MO, C_AQ, C_AK, C_AV, C_GM, C_GA = 0, 512, 1024, 2048, 2064, 3088, 4112, 5136, 6160, 8208


class Prog:
    COMPUTE = ("pe", "act", "dve", "pool")

    def __init__(self):
        self.nc = bass.Bass("TRN2", target_bir_lowering=False)
        self.stack = contextlib.ExitStack()
        self.ops = {e: [] for e in ("pe", "act", "dve", "pool", "sp")}
        self.cnt = {e: 0 for e in self.COMPUTE}
        self.last_w = {}
        self.readers = {}
        self.waited = {e: {} for e in self.ops}
        self.dma_cnt = {}
        self.semnames = ["c_" + e for e in self.COMPUTE]
        self.out_tokens = []
        self._n = 0
        self.cur = self.stack
        self.pending = {e: {} for e in self.ops}

    def dram(self, name, shape, dtype, kind):
        return self.nc.dram_tensor(name, list(shape), dtype, kind=kind).ap()

    def sbuf(self, shape, dtype, name=None):
        self._n += 1
        return self.cur.enter_context(self.nc.sbuf_tensor(name or f"sb{self._n}", list(shape), dtype))

    def psum(self, shape, dtype=F32, name=None):
        self._n += 1
        return self.cur.enter_context(self.nc.psum_tensor(name or f"ps{self._n}", list(shape), dtype))

    @contextlib.contextmanager
    def scope(self):
        prev = self.cur
        self.cur = contextlib.ExitStack()
        try:
            yield
        finally:
            self.cur.close()
            self.cur = prev
            self.fence()

    def fence(self):
        toks = {"c_" + e: self.cnt[e] for e in self.COMPUTE if self.cnt[e] > 0}
        toks.update({sn: v for sn, v in self.dma_cnt.items() if v > 0})
        for e in self.ops:
            self.pending[e] = dict(toks)

    def _deps(self, eng, reads, writes):
        deps = set()
        for k in reads:
            t = self.last_w.get(k)
            if t is not None:
                deps.add(t)
        for k in writes:
            t = self.last_w.get(k)
            if t is not None:
                deps.add(t)
            for r in self.readers.get(k, ()):
                deps.add(r)
        if self.pending[eng]:
            deps |= set(self.pending[eng].items())
            self.pending[eng] = {}
        waits = []
        for (sn, val) in deps:
            if sn == "c_pe" and eng == "pe":
                continue
            if sn in self.dma_cnt:
                val = self.dma_cnt[sn]
            if self.waited[eng].get(sn, 0) >= val:
                continue
            waits.append((sn, val))
        best = {}
        for sn, val in waits:
            best[sn] = max(best.get(sn, 0), val)
        for sn, val in best.items():
            self.waited[eng][sn] = val
        return sorted(best.items())

    def _commit(self, tok, reads, writes):
        for k in writes:
            self.last_w[k] = tok
            self.readers[k] = []
        for k in reads:
            if k in writes:
                continue
            self.readers.setdefault(k, []).append(tok)

    def op(self, eng, fn, reads=(), writes=()):
        reads, writes = tuple(reads), tuple(writes)
        waits = self._deps(eng, reads, writes)
        self.cnt[eng] += 1
        tok = ("c_" + eng, self.cnt[eng])
        self.ops[eng].append((waits, fn, tok[0], 1))
        self._commit(tok, reads, writes)
        return tok

    def dma(self, q, out, in_, sem, reads=(), writes=(), is_out=False, fn=None):
        reads, writes = tuple(reads), tuple(writes)
        if q == "pool":
            self._pq = getattr(self, "_pq", 0) + 1
            writes = writes + (("_poolq", self._pq % 4),)
        waits = self._deps(q, reads, writes)
        if sem not in self.dma_cnt:
            self.dma_cnt[sem] = 0
            self.semnames.append(sem)
        self.dma_cnt[sem] += 16
        tok = (sem, self.dma_cnt[sem])
        if fn is None:
            fn = lambda e, o=out, i=in_: e.dma_start(out=o, in_=i)
        self.ops[q].append((waits, fn, sem, 16))
        self._commit(tok, reads, writes)
        if is_out:
            self.out_tokens.append(tok)
        return tok

    def mm(self, out, lhsT, rhs, start, stop, reads, writes):
        return self.op("pe", lambda e: e.matmul(out, lhsT, rhs, start=start, stop=stop), reads, writes)

    def act(self, out, in_, func, reads, writes, bias=None, scale=None, eng="act"):
        kw = {}
        if bias is not None:
            kw["bias"] = bias
        if scale is not None:
            kw["scale"] = scale
        return self.op(eng, lambda e: e.activation(out=out, in_=in_, func=func, **kw), reads, writes)

    def ts(self, eng, out, in0, s1, s2, op0, op1, reads, writes):
        if s2 is None:
            return self.op(eng, lambda e: e.tensor_scalar(out=out, in0=in0, scalar1=s1, scalar2=None, op0=op0), reads, writes)
        return self.op(eng, lambda e: e.tensor_scalar(out=out, in0=in0, scalar1=s1, scalar2=s2, op0=op0, op1=op1), reads, writes)

    def stt(self, eng, out, in0, scalar, in1, op0, op1, reads, writes):
        return self.op(eng, lambda e: e.scalar_tensor_tensor(out=out, in0=in0, scalar=scalar, in1=in1, op0=op0, op1=op1), reads, writes)

    def tt(self, eng, out, in0, in1, op, reads, writes):
        return self.op(eng, lambda e: e.tensor_tensor(out=out, in0=in0, in1=in1, op=op), reads, writes)

    def copy(self, eng, out, in_, reads, writes):
        if eng == "act":
            return self.op(eng, lambda e: e.activation(out=out, in_=in_, func=AF.Copy), reads, writes)
        return self.op(eng, lambda e: e.tensor_copy(out=out, in_=in_), reads, writes)

    def memset(self, eng, ap, val, writes):
        return self.op(eng, lambda e: e.memset(ap, val), (), writes)

    def build(self):
        nc = self.nc
        fin = {}
        for sn, val in self.out_tokens:
            fin[sn] = max(fin.get(sn, 0), val)
        sems = {}
        for sn in self.semnames:
            sems[sn] = self.stack.enter_context(nc.semaphore(sn))
        ops = self.ops

        def emit(name, e):
            for waits, fn, sn, inc in ops[name]:
                for wsn, val in waits:
                    e.wait_ge(sems[wsn], val)
                fn(e).then_inc(sems[sn], inc)

        with nc.Block() as block:
            @block.tensor
            def _(e):
                emit("pe", e)

            @block.scalar
            def _(e):
                emit("act", e)

            @block.vector
            def _(e):
                emit("dve", e)

            @block.gpsimd
            def _(e):
                emit("pool", e)

            @block.sync
            def _(e):
                emit("sp", e)
                for sn, val in sorted(fin.items()):
                    e.wait_ge(sems[sn], val)
        self.stack.close()
        return nc


def _run(prog, in_maps):
    nc = prog.build()
    res = run_bass_kernel_spmd(nc, in_maps, core_ids=list(range(NCORES)))
    return res.results


def _pk(v):
    v = np.asarray(v, np.float32).reshape(-1, 128)
    return np.ascontiguousarray(v.T)


MODC = 6 * D // NCORES


def build_mod():
    p = Prog()
    c_in = p.dram("c_pk", [128, KC], F32, "ExternalInput")
    w_in = p.dram("ada_w", [DEPTH * 3, 128, KC, 512], F32, "ExternalInput")
    b_in = p.dram("ada_b", [1, DEPTH * MODC], F32, "ExternalInput")
    out = p.dram("mod", [1, DEPTH * MODC], F32, "ExternalOutput")
    c_sb = p.sbuf([128, KC], F32)
    ca = p.sbuf([128, KC], F32)
    b_sb = p.sbuf([1, DEPTH * MODC], F32)
    o_sb = p.sbuf([1, DEPTH * MODC], F32)
    wbuf = [p.sbuf([128, KC, 512], F32) for _ in range(3)]
    ps = [p.psum([128, 512]) for _ in range(2)]
    p.dma("sp", c_sb[:, :], c_in[:, :], "ld_c", writes=["c"])
    p.dma("sp", b_sb[:, :], b_in[:, :], "ld_b", writes=["b"])
    p.act(ca[:, :], c_sb[:, :], AF.Silu, ["c"], ["ca"])
    for j in range(DEPTH * 3):
        wb = wbuf[j % 3]
        p.dma("sp" if j % 2 == 0 else "pool", wb[:, :, :], w_in[j], f"ld_w{j % 3}", writes=[("w", j % 3)])
        pj = ps[j % 2]
        for k in range(KC):
            p.mm(pj[0:1, :], ca[:, k:k + 1], wb[:, k, :], k == 0, k == KC - 1, ["ca", ("w", j % 3)], [("ps", j % 2)])
        p.tt("dve", o_sb[0:1, j * 512:(j + 1) * 512], pj[0:1, :], b_sb[0:1, j * 512:(j + 1) * 512], ALU.add,
             [("ps", j % 2), "b"], [("o", j)])
    p.dma("sp", out[:, :], o_sb[:, :], "st_o", reads=[("o", j) for j in range(DEPTH * 3)], is_out=True)
    return p


def run_mod(c, ada_w, ada_b):
    c_pk = _pk(c.reshape(-1))
    in_maps = []
    for i in range(NCORES):
        w = ada_w[:, :, i * MODC:(i + 1) * MODC]
        w = w.reshape(DEPTH, KC, 128, 3, 512).transpose(0, 3, 2, 1, 4)
        in_maps.append({"c_pk": c_pk, "ada_w": np.ascontiguousarray(w.reshape(DEPTH * 3, 128, KC, 512)),
                        "ada_b": np.ascontiguousarray(ada_b[:, i * MODC:(i + 1) * MODC].reshape(1, -1))})
    res = _run(build_mod(), in_maps)
    mod = np.concatenate([r["mod"].reshape(DEPTH, MODC) for r in res], axis=1)
    return mod


def _load_xT(p, xT_d, x_sb, q="sp"):
    for g in range(4):
        p.dma(q, x_sb[:, 4 * g:4 * g + 4, :], xT_d.rearrange("(k p) t -> p k t", p=128)[:, 4 * g:4 * g + 4, :],
              f"ld_x{g}", writes=[("x", k) for k in range(4 * g, 4 * g + 4)])


def _norm_mod(p, getx, hT, gpk, scpk, shpk, ones_f, ps_ss, tagp, hook=None, tagpar=None):
    T = TPC
    tagpar = tagpar or tagp
    gs = p.sbuf([128, KC], F32)
    p.stt("dve", gs[:, :], scpk, 1.0, gpk, ALU.add, ALU.mult, [tagpar], [tagp + "gs"])
    sq = [p.sbuf([128, T], F32) for _ in range(2)]
    for k in range(KC):
        s = sq[k % 2]
        xa, xk = getx(k, 0)
        p.act(s[:, :], xa, AF.Square, [xk], [(tagp + "sq", k % 2)])
        for h in range(2):
            p.mm(ps_ss[h][:, :], ones_f[:, :], s[:, h * 512:(h + 1) * 512], k == 0, k == KC - 1,
                 [(tagp + "sq", k % 2), "ones_f"], [(tagp + "ss", h)])
    rstd = p.sbuf([128, T], F32)
    for h in range(2):
        p.act(rstd[:, h * 512:(h + 1) * 512], ps_ss[h][:, :], AF.Sqrt, [(tagp + "ss", h)], [(tagp + "rstd", h)],
              bias=EPS, scale=1.0 / D)
        p.op("dve", lambda e, h=h: e.reciprocal(out=rstd[:, h * 512:(h + 1) * 512], in_=rstd[:, h * 512:(h + 1) * 512]),
             [(tagp + "rstd", h)], [(tagp + "rstd", h)])
    tmp = [p.sbuf([128, T], F32) for _ in range(2)]
    hf = sq
    for k in range(KC):
        t = tmp[k % 2]
        f = hf[k % 2]
        xa, xk = getx(k, 1)
        p.stt("dve", t[:, :], xa, gs[:, k:k + 1], rstd[:, :], ALU.mult, ALU.mult,
              [xk, tagp + "gs", (tagp + "rstd", 0), (tagp + "rstd", 1)], [(tagp + "tmp", k % 2)])
        p.act(f[:, :], t[:, :], AF.Identity, [(tagp + "tmp", k % 2), tagpar], [(tagp + "sq", k % 2)], bias=shpk[:, k:k + 1])
        p.copy("pool", hT[:, k, :], f[:, :], [(tagp + "sq", k % 2)], [(tagp + "h", k)])
        if hook is not None:
            hook(k, f, (tagp + "sq", k % 2))


NFM = 24
ZF_ROWS = NFM * 128 + 16


def build_A(combine):
    p = Prog()
    T = TPC
    xT_d = p.dram("xT", [D, T], F32, "ExternalInput")
    par_d = p.dram("par", [128, 4 * KC], F32, "ExternalInput")
    wfm_d = p.dram("wfm", [NFM, 128, KC, 128], F32, "ExternalInput")
    wg_d = p.dram("wg", [128, KC, 16], F32, "ExternalInput")
    wtm_d = p.dram("wtm", [4, 128, KC, 512], F32, "ExternalInput")
    zf_d = p.dram("zf", [ZF_ROWS, T], F32, "ExternalOutput")
    zv_d = p.dram("zv", [T, 2048], F32, "ExternalOutput")
    if combine:
        yT_d = p.dram("yT", [2, D, T], F32, "ExternalInput")
        xo_d = p.dram("xo", [D, T], F32, "ExternalOutput")

    x_sb = p.sbuf([128, KC, T], F32)
    hT = p.sbuf([128, KC, T], BF16)
    par = p.sbuf([128, 4 * KC], F32)
    ones_f = p.sbuf([128, 128], F32)
    ps_ss = [p.psum([128, 512]) for _ in range(2)]
    ps_mm = [p.psum([128, 512]) for _ in range(4)]
    p.memset("pool", ones_f[:, :], 1.0, ["ones_f"])
    p.dma("sp", par[:, :], par_d[:, :], "ld_par", writes=["par"])
    _load_xT(p, xT_d, x_sb)

    if combine:
        ybuf = [p.sbuf([128, 2, T], F32) for _ in range(2)]
        for k in range(KC):
            yb = ybuf[k % 2]
            p.dma("sp", yb[:, :, :], yT_d.rearrange("s (k p) t -> p k s t", p=128)[:, k, :, :], f"ld_y{k % 2}",
                  writes=[("y", k % 2)])
            p.tt("pool", yb[:, 0, :], yb[:, 0, :], yb[:, 1, :], ALU.add, [("y", k % 2)], [("y", k % 2)])
            p.stt("dve", x_sb[:, k, :], yb[:, 0, :], par[:, 3 * KC + k:3 * KC + k + 1], x_sb[:, k, :], ALU.mult, ALU.add,
                  [("y", k % 2), ("x", k), "par"], [("x", k)])
        for g in range(4):
            p.dma("sp", xo_d.rearrange("(k p) t -> p k t", p=128)[:, 4 * g:4 * g + 4, :], x_sb[:, 4 * g:4 * g + 4, :],
                  f"st_x{g}", reads=[("x", k) for k in range(4 * g, 4 * g + 4)], is_out=True)

    wg = p.sbuf([128, KC, 16], F32)
    p.dma("sp", wg[:, :, :], wg_d[:, :, :], "ld_wg", writes=["wg"])
    ps_g = [p.psum([128, 512]) for _ in range(2)]

    def gate_hook(k, f, fkey):
        for h in range(2):
            p.mm(ps_g[h][0:16, :], wg[:, k, :], f[:, h * 512:(h + 1) * 512], k == 0, k == KC - 1, ["wg", fkey], [("psg", h)])

    _norm_mod(p, lambda k, ps_: (x_sb[:, k, :], ("x", k)), hT, par[:, 0:KC], par[:, KC:2 * KC], par[:, 2 * KC:3 * KC],
              ones_f, ps_ss, "par", hook=gate_hook)
    hkeys = [("parh", k) for k in range(KC)]

    wfm = [p.sbuf([128, KC, 128], BF16) for _ in range(3)]
    stg = [p.sbuf([128, T], F32) for _ in range(2)]
    mmi = 0
    for b in range(NFM + 1):
        isg = b == NFM
        M = 16 if isg else 128
        if not isg:
            w = wfm[b % 3]
            p.dma("pool", w[:, :, :], wfm_d[b], f"ld_wfm{b % 3}", writes=[("wfm", b % 3)])
            wkey = ("wfm", b % 3)
        st = stg[b % 2]
        for h in range(2):
            if isg:
                ps, pk = ps_g[h], ("psg", h)
            else:
                ps = ps_mm[mmi % 2]
                pk = ("psmm", mmi % 2)
                mmi += 1
                for k in range(KC):
                    p.mm(ps[0:M, :], w[:, k, :], hT[:, k, h * 512:(h + 1) * 512], k == 0, k == KC - 1, [wkey, hkeys[k]], [pk])
            p.copy("act" if h == 0 else "dve", st[0:M, h * 512:(h + 1) * 512], ps[0:M, :], [pk], [("stg", b % 2, h)])
        p.dma("sp", zf_d[b * 128:b * 128 + M, :], st[0:M, :], f"st_zf{b % 2}",
              reads=[("stg", b % 2, 0), ("stg", b % 2, 1)], is_out=True)

    wtm = [p.sbuf([128, KC, 512], BF16) for _ in range(2)]
    stv = [p.sbuf([128, 512], F32) for _ in range(3)]
    si = 0
    for s4 in range(4):
        w = wtm[s4 % 2]
        for g in range(4):
            p.dma("pool", w[:, 4 * g:4 * g + 4, :], wtm_d[s4][:, 4 * g:4 * g + 4, :], f"ld_wtm{s4 % 2}",
                  writes=[("wtm", s4 % 2, g)])
        for tt_ in range(T // 128):
            ps = ps_mm[mmi % 4]
            pk = ("psmm", mmi % 4)
            mmi += 1
            for k in range(KC):
                p.mm(ps[:, :], hT[:, k, tt_ * 128:(tt_ + 1) * 128], w[:, k, :], k == 0, k == KC - 1,
                     [("wtm", s4 % 2, k // 4), hkeys[k]], [pk])
            sv = stv[si % 3]
            p.copy("act" if si % 2 == 0 else "dve", sv[:, :], ps[:, :], [pk], [("stv", si % 3)])
            p.dma("sp", zv_d[tt_ * 128:(tt_ + 1) * 128, s4 * 512:(s4 + 1) * 512], sv[:, :], f"st_zv{si % 3}",
                  reads=[("stv", si % 3)], is_out=True)
            si += 1
    return p


def _blk(w, cols):
    return np.ascontiguousarray(w[:, cols].reshape(KC, 128, -1).transpose(1, 0, 2))


def prep_A_weights(w_in_l):
    fm_cols = []
    for h in range(4):
        fm_cols.append(np.arange(C_MQ + h * 128, C_MQ + (h + 1) * 128))
    for h in range(4):
        fm_cols.append(np.arange(C_MK + h * 128, C_MK + (h + 1) * 128))
    for h in range(8):
        fm_cols.append(np.arange(C_AQ + h * 128, C_AQ + (h + 1) * 128))
    for h in range(8):
        fm_cols.append(np.arange(C_AK + h * 128, C_AK + (h + 1) * 128))
    wfm = np.stack([_blk(w_in_l, c) for c in fm_cols])
    wg = _blk(w_in_l, np.arange(C_MG, C_MG + 16))
    tm = [np.arange(C_MV + j * 512, C_MV + (j + 1) * 512) for j in range(2)] + \
         [np.arange(C_AV + j * 512, C_AV + (j + 1) * 512) for j in range(2)]
    wtm = np.stack([_blk(w_in_l, c) for c in tm])
    return wfm, wg, wtm


def run_A(xT_sh, mod_l, norm1_g_l, w_in_l, g2prev=None, yT_sh=None):
    combine = yT_sh is not None
    sh1, sc1 = mod_l[0:D], mod_l[D:2 * D]
    par = np.concatenate([_pk(norm1_g_l), _pk(sc1), _pk(sh1), _pk(g2prev if combine else np.zeros(D, np.float32))], axis=1)
    wfm, wg, wtm = prep_A_weights(w_in_l)
    in_maps = []
    for i in range(NCORES):
        m = {"xT": xT_sh[i], "par": par, "wfm": wfm, "wg": wg, "wtm": wtm}
        if combine:
            m["yT"] = yT_sh[i]
        in_maps.append(m)
    res = _run(build_A(combine), in_maps)
    return [r["zf"] for r in res], [r["zv"] for r in res], ([r["xo"] for r in res] if combine else xT_sh)


def rope_tables_np():
    pos = np.arange(S, dtype=np.float32)
    inv = np.float32(500000.0) ** (-(np.arange(0, 16, 2, dtype=np.float32)) / np.float32(16))
    ang = pos[:, None] * inv[None, :].astype(np.float32)
    cos = np.cos(ang).astype(np.float32).T
    sin = np.sin(ang).astype(np.float32).T
    return np.ascontiguousarray(np.concatenate([cos, cos], 0)), np.ascontiguousarray(np.concatenate([sin, sin], 0))


def const_mats():
    blk64 = np.zeros((128, 128), np.float32)
    blk64[:64, :64] = 1
    blk64[64:, 64:] = 1
    pm = np.zeros((128, 128), np.float32)
    for base in (0, 64):
        for m in range(8):
            pm[base + m + 8, base + m] = -1.0
            pm[base + m, base + m + 8] = 1.0
    return blk64, pm


def _attention(p, d, banks):
    QB = 512
    NQB = S // QB
    NKT = S // 128
    blk64 = p.sbuf([128, 128], F32)
    pm = p.sbuf([128, 128], F32)
    ones_f = p.sbuf([128, 128], F32)
    ones_bf = p.sbuf([128, 128], BF16)
    para = p.sbuf([128, 8], F32)
    lam = p.sbuf([128, 256], F32)
    sm = p.sbuf([128, 16], F32)
    p.dma("sp", blk64[:, :], d["blk64"][:, :], "ld_c0", writes=["blk64"])
    p.dma("sp", pm[:, :], d["pm"][:, :], "ld_c1", writes=["pm"])
    p.dma("sp", para[:, :], d["para"][:, :], "ld_c2", writes=["para"])
    p.dma("sp", lam[:, :], d["lam"][:, :], "ld_c3", writes=["lam"])
    p.memset("pool", ones_f[:, :], 1.0, ["ones_f"])
    p.memset("pool", ones_bf[:, :], 1.0, ["ones_bf"])
    lp = p.sbuf([128, 128], F32)
    p.tt("dve", lp[:, 0:64], lam[:, 0:64], lam[:, 64:128], ALU.mult, ["lam"], ["lp"])
    p.tt("dve", lp[:, 64:128], lam[:, 128:192], lam[:, 192:256], ALU.mult, ["lam", "lp"], ["lp"])
    p.op("dve", lambda e: e.reduce_sum(out=sm[:, 0:1], in_=lp[:, 0:64], axis=mybir.AxisListType.X), ["lp"], ["sm"])
    p.op("dve", lambda e: e.reduce_sum(out=sm[:, 1:2], in_=lp[:, 64:128], axis=mybir.AxisListType.X), ["lp", "sm"], ["sm"])
    p.act(sm[:, 2:4], sm[:, 0:2], AF.Exp, ["sm"], ["sm"])
    p.tt("dve", sm[:, 4:5], sm[:, 3:4], sm[:, 2:3], ALU.subtract, ["sm"], ["sm"])
    p.ts("dve", sm[:, 4:5], sm[:, 4:5], para[:, 3:4], None, ALU.add, None, ["sm", "para"], ["sm"])
    p.ts("dve", sm[:, 5:6], para[:, 0:1], 0.125, None, ALU.mult, None, ["para", "sm"], ["sm"])
    p.ts("dve", sm[:, 6:7], para[:, 1:2], 1.0, None, ALU.mult, None, ["para", "sm"], ["sm"])
    p.ts("dve", sm[:, 7:8], para[:, 2:3], para[:, 4:5], None, ALU.mult, None, ["para", "sm"], ["sm"])

    qn = p.sbuf([128, S], BF16)
    kn = p.sbuf([128, S], BF16)
    v_bf = p.sbuf([128, NKT, 128], BF16)
    for g in range(4):
        p.dma("pool", v_bf[:, 16 * g:16 * g + 16, :], d["av"].rearrange("(t p) c -> p t c", p=128)[:, 16 * g:16 * g + 16, :],
              "ld_v", writes=[("v", g)])
    psS = [[banks[0], banks[1]], [banks[2], banks[3]]]
    psO = [banks[3], banks[4]]
    psD = banks[5]
    _bi = {(0, 0): 0, (0, 1): 1, (1, 0): 2, (1, 1): 3}
    skey = lambda c, i: ("bank", _bi[(c, i)])

    NPT = 4
    PT = [[p.sbuf([128, QB], BF16) for _ in range(NPT)] for _ in range(2)]
    r1 = p.sbuf([128, QB], F32)
    r2 = p.sbuf([128, QB], F32)
    o1 = p.sbuf([128, QB], F32)
    o2 = p.sbuf([128, QB], F32)
    ob = [p.sbuf([128, QB], F32) for _ in range(2)]
    osq = p.sbuf([128, QB], F32)
    ors = p.sbuf([128, QB], F32)
    yst = [p.sbuf([128, QB], F32) for _ in range(2)]
    rrow = p.sbuf([64, QB], F32)
    esel = [p.sbuf([128, 64], BF16) for _ in range(2)]
    for c in range(2):
        p.memset("pool", esel[c][:, :], 0.0, ["esel"])
        p.memset("pool", esel[c][:, 32 * c:32 * c + 1], 1.0, ["esel"])
    pre = p.scope()
    pre.__enter__()
    raw = [p.sbuf([128, QB], F32) for _ in range(2)]
    sqb = [p.sbuf([128, QB], F32) for _ in range(2)]
    rsb = [p.sbuf([128, QB], F32) for _ in range(2)]
    qnf = [p.sbuf([128, QB], F32) for _ in range(2)]
    cst = [p.sbuf([128, QB], F32) for _ in range(2)]
    snt = [p.sbuf([128, QB], F32) for _ in range(2)]
    t1b = [p.sbuf([128, QB], F32) for _ in range(2)]
    t2b = [p.sbuf([128, QB], F32) for _ in range(2)]
    it = 0
    for which, src_d, dst, gcol in (("q", d["aqT"], qn, 5), ("k", d["akT"], kn, 6)):
        for b in range(NQB):
            r = it % 2
            sl = slice(b * QB, (b + 1) * QB)
            p.dma("sp", raw[r][:, :], src_d[:, sl], f"ld_raw{r}", writes=[("raw", r)])
            for base in (0, 64):
                p.dma("sp", cst[r][base:base + 16, :], d["cos"][:, sl], f"ld_cs{r}", writes=[("cs", r, base)])
                p.dma("sp", snt[r][base:base + 16, :], d["sin"][:, sl], f"ld_sn{r}", writes=[("sn", r, base)])
            p.act(sqb[r][:, :], raw[r][:, :], AF.Square, [("raw", r)], [("sq", r)])
            p.mm(psS[0][r][:, :], blk64[:, :], sqb[r][:, :], True, True, ["blk64", ("sq", r)], [skey(0, r)])
            p.act(rsb[r][:, :], psS[0][r][:, :], AF.Sqrt, [skey(0, r)], [("rs", r)], bias=EPS, scale=1.0 / 64)
            p.op("dve", lambda e, r=r: e.reciprocal(out=rsb[r][:, :], in_=rsb[r][:, :]), [("rs", r)], [("rs", r)])
            p.stt("dve", qnf[r][:, :], raw[r][:, :], sm[:, gcol:gcol + 1], rsb[r][:, :], ALU.mult, ALU.mult,
                  [("raw", r), ("rs", r), "sm"], [("qnf", r)])
            p.mm(psS[1][r][:, :], pm[:, :], qnf[r][:, :], True, True, ["pm", ("qnf", r)], [skey(1, r)])
            p.copy("act", dst[:, sl], qnf[r][:, :], [("qnf", r)], [(which + "n", b)])
            for base in (0, 64):
                rows = slice(base, base + 16)
                p.tt("pool", t1b[r][rows, :], qnf[r][rows, :], cst[r][rows, :], ALU.mult,
                     [("qnf", r), ("cs", r, base)], [("t1", r, base)])
                p.tt("dve", t2b[r][rows, :], psS[1][r][rows, :], snt[r][rows, :], ALU.mult,
                     [skey(1, r), ("sn", r, base)], [("t2", r, base)])
                p.tt("dve", dst[rows, sl], t1b[r][rows, :], t2b[r][rows, :], ALU.add,
                     [("t1", r, base), ("t2", r, base), (which + "n", b)], [(which + "n", b)])
            it += 1

    pre.__exit__(None, None, None)

    ring = [0]

    def qk(qb, kt, c, st):
        b = ring[0] % 3
        ring[0] += 1
        st[(kt, c)] = b
        rows = slice(c * 64, (c + 1) * 64)
        p.mm(banks[b][:, :], kn[rows, kt * 128:(kt + 1) * 128], qn[rows, qb * QB:(qb + 1) * QB], True, True,
             [("kn", kt // 4), ("qn", qb)], [("bank", b)])

    stepc = [0]

    def step(qb, kt, st):
        if kt == 0:
            qk(qb, 0, 0, st)
            qk(qb, 0, 1, st)
        if kt + 1 < NKT:
            qk(qb, kt + 1, 0, st)
        sl = stepc[0] % NPT
        stepc[0] += 1
        for c in range(2):
            b = st[(kt, c)]
            p.act(PT[c][sl][:, :], banks[b][:, :], AF.Exp, [("bank", b)], [("PT", c, sl)])
        if kt + 1 < NKT:
            qk(qb, kt + 1, 1, st)
        for c in range(2):
            p.mm(psO[c][:, :], v_bf[:, kt, :], PT[c][sl][:, :], kt == 0, kt == NKT - 1, [("v", kt // 16), ("PT", c, sl)], [("bank", 3 + c)])
        p.mm(psD[0:64, :], esel[0][:, :], PT[0][sl][:, :], kt == 0, False, ["esel", ("PT", 0, sl)], [("bank", 5)])
        p.mm(psD[0:64, :], esel[1][:, :], PT[1][sl][:, :], False, kt == NKT - 1, ["esel", ("PT", 1, sl)], [("bank", 5)])

    def epilogue(qb):
        o = ob[qb % 2]
        for c in range(2):
            p.op("dve", lambda e, c=c: e.reciprocal(out=rrow[32 * c:32 * c + 1, :], in_=psD[32 * c:32 * c + 1, :]), [("bank", 5)], [("rrow", c)])
            p.mm(banks[c][:, :], ones_f[32 * c:32 * c + 1, :], rrow[32 * c:32 * c + 1, :], True, True, ["ones_f", ("rrow", c)], [("bank", c)])
        p.copy("act", r1[:, :], banks[0][:, :], [("bank", 0)], ["r1"])
        p.copy("act", r2[:, :], banks[1][:, :], [("bank", 1)], ["r2"])
        p.tt("dve", o1[:, :], psO[0][:, :], r1[:, :], ALU.mult, [("bank", 3), "r1"], ["o1"])
        p.tt("dve", o2[:, :], psO[1][:, :], r2[:, :], ALU.mult, [("bank", 4), "r2"], ["o2"])
        p.stt("dve", o[:, :], o2[:, :], sm[:, 4:5], o1[:, :], ALU.mult, ALU.add, ["o1", "o2", "sm"], [("o", qb % 2)])
        p.tt("pool", osq[:, :], o[:, :], o[:, :], ALU.mult, [("o", qb % 2)], ["osq"])
        p.mm(banks[2][:, :], ones_f[:, :], osq[:, :], True, True, ["ones_f", "osq"], [("bank", 2)])
        p.act(ors[:, :], banks[2][:, :], AF.Sqrt, [("bank", 2)], ["ors"], bias=EPS, scale=1.0 / 128)
        p.op("dve", lambda e: e.reciprocal(out=ors[:, :], in_=ors[:, :]), ["ors"], ["ors"])
        y = yst[qb % 2]
        p.stt("dve", y[:, :], o[:, :], sm[:, 7:8], ors[:, :], ALU.mult, ALU.mult, [("o", qb % 2), "sm", "ors"], [("y", qb % 2)])
        p.dma("sp", d["yaT"][:, qb * QB:(qb + 1) * QB], y[:, :], f"st_y{qb % 2}", reads=[("y", qb % 2)], is_out=True)

    return step, epilogue, NQB, NKT


def build_B():
    p = Prog()
    d = {}
    d["aqT"] = p.dram("aqT", [128, S], F32, "ExternalInput")
    d["akT"] = p.dram("akT", [128, S], F32, "ExternalInput")
    d["av"] = p.dram("av", [S, 128], F32, "ExternalInput")
    d["para"] = p.dram("para", [128, 8], F32, "ExternalInput")
    d["lam"] = p.dram("lam", [128, 256], F32, "ExternalInput")
    d["cos"] = p.dram("cos", [16, S], F32, "ExternalInput")
    d["sin"] = p.dram("sin", [16, S], F32, "ExternalInput")
    d["blk64"] = p.dram("blk64", [128, 128], F32, "ExternalInput")
    d["pm"] = p.dram("pm", [128, 128], F32, "ExternalInput")
    d["yaT"] = p.dram("yaT", [128, S], F32, "ExternalOutput")
    _mlstm_decl(p, d)
    banks = [p.psum([128, 512]) for _ in range(7)]
    bk7 = p.psum([128, 512])
    psT = bk7[:, 384:448].bitcast(BF16)
    chunk_stages, NT = _mlstm(p, d, banks, bk7, psT)
    step, epilogue, NQB, NKT = _attention(p, d, banks)
    per = (NQB * NKT) // NT
    pending = {}
    n = 0
    for qb in range(NQB):
        st = {}
        for kt in range(NKT):
            if n % per == 0 and n // per < NT:
                stages = chunk_stages(n // per)
                for j, fn in enumerate(stages):
                    pending.setdefault(n + 1 + 2 * j, []).append(fn)
            for fn in pending.pop(n, []):
                fn()
            step(qb, kt, st)
            n += 1
        epilogue(qb)
    for k in sorted(pending):
        for fn in pending[k]:
            fn()
    return p


def mlstm_consts():
    tri = np.triu(np.ones((128, 128), np.float32))
    maskp = np.where(np.arange(128)[:, None] <= np.arange(128)[None, :], 0.0, 30000.0).astype(np.float32)
    ident = np.eye(128, dtype=np.float32)
    return tri, maskp, ident


def _mlstm_decl(p, d):
    d["mqT"] = p.dram("mqT", [128, S], F32, "ExternalInput")
    d["mkT"] = p.dram("mkT", [128, S], F32, "ExternalInput")
    d["mv"] = p.dram("mv", [S, 256], F32, "ExternalInput")
    d["gtm"] = p.dram("gtm", [128, 128], F32, "ExternalInput")
    d["mpar"] = p.dram("mpar", [128, 12], F32, "ExternalInput")
    d["tri"] = p.dram("tri", [128, 128], F32, "ExternalInput")
    d["maskp"] = p.dram("maskp", [128, 128], F32, "ExternalInput")
    d["ident"] = p.dram("ident", [128, 128], F32, "ExternalInput")
    d["hmT"] = p.dram("hmT", [256, S], F32, "ExternalOutput")


def _scan_free(p, eng, bufs, n, op, key):
    cur, nxt = bufs[0], bufs[1]
    sh = 1
    while sh < n:
        p.tt(eng, nxt[:, sh:n], cur[:, sh:n], cur[:, 0:n - sh], op, [key], [key])
        p.copy(eng, nxt[:, 0:sh], cur[:, 0:sh], [key], [key])
        cur, nxt = nxt, cur
        sh *= 2
    return cur


def _mlstm(p, d, banks, bk7, psTs):
    NT = S // 128
    LNS = math.log(128.0 ** -0.5)
    X = mybir.AxisListType.X
    mpar = p.sbuf([128, 12], F32)
    tri = p.sbuf([128, 128], F32)
    maskp = p.sbuf([128, 128], F32)
    ident = p.sbuf([128, 128], F32)
    ident_bf = p.sbuf([128, 128], BF16)
    ones_f = p.sbuf([128, 128], F32)
    ones_bf = p.sbuf([128, 128], BF16)
    gtm = p.sbuf([128, 128], F32)
    for nm, t in (("mpar", mpar), ("tri", tri), ("maskp", maskp), ("ident", ident), ("gtm", gtm)):
        p.dma("sp", t[:, :], d[nm][:, :], "ld_" + nm, writes=[nm])
    p.dma("pool", ident_bf[:, :], d["ident"][:, :], "ld_idbf", writes=["ident_bf"])
    p.memset("pool", ones_f[:, :], 1.0, ["ones_f"])
    p.memset("pool", ones_bf[:, :], 1.0, ["ones_bf"])
    v_bf = p.sbuf([128, NT, 256], BF16)
    for g in range(4):
        p.dma("pool", v_bf[:, 16 * g:16 * g + 16, :], d["mv"].rearrange("(t p) c -> p t c", p=128)[:, 16 * g:16 * g + 16, :],
              "ld_mv", writes=[("mv", g)])
    bank = banks

    a_tm = p.sbuf([128, 64], F32)
    A_G = p.sbuf([64, 128], F32)
    lb_G = p.sbuf([64, 128], F32)
    aendp = p.sbuf([128, 64], F32)
    w_tm = p.sbuf([128, 64], F32)
    dec = p.sbuf([128, 64], F32)
    qT = p.sbuf([128, S], BF16)
    kT = p.sbuf([128, S], BF16)
    Cst = [p.sbuf([128, 384], F32) for _ in range(2)]
    Cbf = [p.sbuf([128, 384], BF16) for _ in range(2)]
    sel = [p.sbuf([64, 128], F32) for _ in range(2)]
    ones64 = p.sbuf([64, 128], F32)
    Xb = [p.sbuf([128, 128], F32) for _ in range(2)]
    Eb = [p.sbuf([128, 128], F32) for _ in range(2)]
    Yb = [p.sbuf([128, 128], F32) for _ in range(2)]
    ST = [p.sbuf([128, 128], BF16) for _ in range(2)]
    qp = [p.sbuf([128, 128], BF16) for _ in range(2)]
    kw = [p.sbuf([128, 128], BF16) for _ in range(2)]
    dab = [p.sbuf([128, 128], F32) for _ in range(2)]
    hout = [p.sbuf([128, 2, 128], F32) for _ in range(2)]
    pre = p.scope()
    pre.__enter__()
    i_tm = p.sbuf([128, 64], F32)
    lf = p.sbuf([128, 64], F32)
    p.ts("dve", i_tm[:, :], gtm[:, 0:64], mpar[:, 10:11], None, ALU.add, None, ["gtm", "mpar"], ["i_tm"])
    p.ts("dve", lf[:, :], gtm[:, 64:128], mpar[:, 11:12], None, ALU.add, None, ["gtm", "mpar"], ["lf"])
    p.act(lf[:, :], lf[:, :], AF.Exp, ["lf"], ["lf"], scale=-1.0)
    p.act(lf[:, :], lf[:, :], AF.Ln, ["lf"], ["lf"], bias=1.0)
    p.mm(bank[0][:, 0:64], tri[:, :], lf[:, :], True, True, ["tri", "lf"], [("bank", 0)])
    p.mm(bank[0][:, 64:128], ones_f[:, :], lf[:, :], True, True, ["ones_f", "lf"], [("bank", 0)])
    sc = [p.sbuf([128, 64], F32) for _ in range(2)]
    p.copy("dve", sc[0][:, :], bank[0][:, 64:128], [("bank", 0)], ["sc"])
    incl = _scan_free(p, "dve", sc, 64, ALU.add, "sc")
    nF = p.sbuf([128, 64], F32)
    p.tt("dve", nF[:, :], incl[:, :], bank[0][:, 64:128], ALU.subtract, ["sc", ("bank", 0)], ["nF"])
    p.tt("dve", nF[:, :], nF[:, :], bank[0][:, 0:64], ALU.add, ["nF", ("bank", 0)], ["nF"])
    p.tt("dve", a_tm[:, :], i_tm[:, :], nF[:, :], ALU.add, ["i_tm", "nF"], ["a_tm"])
    p.mm(bank[1][0:64, 0:128], a_tm[:, :], ident[:, :], True, True, ["a_tm", "ident"], [("bank", 1)])
    p.mm(bank[1][0:64, 128:256], nF[:, :], ident[:, :], True, True, ["nF", "ident"], [("bank", 1)])
    cm = [p.sbuf([64, 128], F32) for _ in range(2)]
    nF_G = p.sbuf([64, 128], F32)
    p.copy("dve", cm[0][:, :], bank[1][0:64, 0:128], [("bank", 1)], ["cm"])
    p.copy("dve", nF_G[:, :], bank[1][0:64, 128:256], [("bank", 1)], ["nF_G"])
    cmx = _scan_free(p, "dve", cm, 128, ALU.max, "cm")
    p.ts("dve", cmx[:, :], cmx[:, :], 0.0, None, ALU.max, None, ["cm"], ["cm"])
    p.mm(bank[2][0:1, 0:64], cmx[:, 127:128], ident[0:64, 0:64], True, True, ["cm", "ident"], [("bank", 2)])
    rw = [p.sbuf([1, 64], F32) for _ in range(2)]
    p.copy("dve", rw[0][:, :], bank[2][0:1, 0:64], [("bank", 2)], ["rw"])
    rmax = _scan_free(p, "dve", rw, 64, ALU.max, "rw")
    rsh = p.sbuf([1, 64], F32)
    p.memset("dve", rsh[:, :], 0.0, ["rsh"])
    p.copy("dve", rsh[:, 1:64], rmax[:, 0:63], ["rw", "rsh"], ["rsh"])
    p.mm(bank[2][0:64, 64:65], rsh[0:1, :], ones_f[0:1, 0:1], True, True, ["rsh", "ones_f"], [("bank", 2)])
    pcol = p.sbuf([64, 1], F32)
    p.copy("dve", pcol[:, :], bank[2][0:64, 64:65], [("bank", 2)], ["pcol"])
    p.ts("dve", A_G[:, :], cmx[:, :], pcol[:, 0:1], None, ALU.max, None, ["cm", "pcol"], ["A_G"])
    p.tt("dve", lb_G[:, :], nF_G[:, :], A_G[:, :], ALU.subtract, ["nF_G", "A_G"], ["lb_G"])
    p.act(lb_G[:, :], lb_G[:, :], AF.Exp, ["lb_G"], ["lb_G"])
    arep = p.sbuf([64, 128], F32)
    p.memset("dve", arep[:, :], 0.0, ["arep"])
    p.ts("dve", arep[:, :], arep[:, :], A_G[:, 127:128], None, ALU.add, None, ["arep", "A_G"], ["arep"])
    p.mm(bank[3][:, 0:64], arep[:, :], ident[0:64, 0:64], True, True, ["arep", "ident"], [("bank", 3)])
    aend = p.sbuf([128, 64], F32)
    p.copy("dve", aend[:, :], bank[3][:, 0:64], [("bank", 3)], ["aend"])
    p.memset("dve", aendp[:, :], 0.0, ["aendp"])
    p.copy("dve", aendp[:, 1:64], aend[:, 0:63], ["aend", "aendp"], ["aendp"])
    p.tt("dve", w_tm[:, :], aend[:, :], a_tm[:, :], ALU.subtract, ["aend", "a_tm"], ["w_tm"])
    p.ts("dve", w_tm[:, :], w_tm[:, :], 0.0, None, ALU.max, None, ["w_tm"], ["w_tm"])
    p.act(w_tm[:, :], w_tm[:, :], AF.Exp, ["w_tm"], ["w_tm"], scale=-1.0)
    p.tt("dve", dec[:, :], aend[:, :], aendp[:, :], ALU.subtract, ["aend", "aendp"], ["dec"])
    p.act(dec[:, :], dec[:, :], AF.Exp, ["dec"], ["dec"], scale=-1.0)

    CH = 2048
    cbuf = [p.sbuf([128, CH + 4], F32) for _ in range(2)]
    cacc = [p.sbuf([128, CH], F32) for _ in range(2)]
    it = 0
    for which, src, dst, c0 in (("q", d["mqT"], qT, 0), ("k", d["mkT"], kT, 5)):
        for g in range(S // CH):
            r = it % 2
            it += 1
            lo = g * CH - 2
            hi = (g + 1) * CH + 2
            if lo < 0:
                p.memset("pool", cbuf[r][:, 0:2], 0.0, [("cbuf", r)])
            if hi > S:
                p.memset("pool", cbuf[r][:, CH + 2:CH + 4], 0.0, [("cbuf", r)])
            slo, shi = max(lo, 0), min(hi, S)
            p.dma("sp", cbuf[r][:, slo - lo:shi - lo], src[:, slo:shi], f"ld_cb{r}", writes=[("cbuf", r)])
            p.ts("dve", cacc[r][:, :], cbuf[r][:, 0:CH], mpar[:, c0:c0 + 1], None, ALU.mult, None, [("cbuf", r), "mpar"], [("cacc", r)])
            for j in range(1, 5):
                p.stt("dve", cacc[r][:, :], cbuf[r][:, j:j + CH], mpar[:, c0 + j:c0 + j + 1], cacc[r][:, :], ALU.mult, ALU.add,
                      [("cbuf", r), ("cacc", r), "mpar"], [("cacc", r)])
            p.act(dst[:, g * CH:(g + 1) * CH], cacc[r][:, :], AF.Silu, [("cacc", r)], [(which + "T", g)])

    pre.__exit__(None, None, None)
    p.memset("pool", ones64[:, :], 1.0, ["ones64"])
    AB = banks[6]
    UH = bk7
    KAB, KUH = ("bank", 6), ("bank", 7)

    def chunk_stages(c):
        r = c % 2
        tsl = slice(c * 128, (c + 1) * 128)
        g4 = c // 16
        prev = (c - 1) % 2
        psT = psTs

        def s1():
            p.ts("pool", sel[r][:, :], ones64[:, :], ident[0:64, c:c + 1], None, ALU.mult, None, ["ones64", "ident"], [("sel", r)])
            p.mm(AB[:, 0:128], sel[r][:, :], A_G[:, :], True, True, [("sel", r), "A_G"], [KAB])
            p.mm(AB[:, 128:256], sel[r][:, :], lb_G[:, :], True, True, [("sel", r), "lb_G"], [KAB])
            p.mm(AB[:, 256:384], kT[:, tsl], qT[:, tsl], True, True, [("kT", g4), ("qT", g4)], [KAB])
            p.op("pe", lambda e: e.transpose(psT[:, :], kT[:, tsl], ident_bf[:, :]), [("kT", g4), "ident_bf"], ["psT"])

        def s2():
            p.ts("dve", Xb[r][:, :], AB[:, 0:128], a_tm[:, c:c + 1], 0.0, ALU.subtract, ALU.max, [KAB, "a_tm"], [("X", r)])
            p.tt("pool", Xb[r][:, :], Xb[r][:, :], maskp[:, :], ALU.add, [("X", r), "maskp"], [("X", r)])
            p.act(Eb[r][:, :], Xb[r][:, :], AF.Exp, [("X", r)], [("E", r)], scale=-1.0, bias=LNS)
            p.tt("dve", ST[r][:, :], AB[:, 256:384], Eb[r][:, :], ALU.mult, [KAB, ("E", r)], [("ST", r)])
            p.ts("dve", Yb[r][:, :], AB[:, 0:128], aendp[:, c:c + 1], None, ALU.subtract, None, [KAB, "aendp"], [("Y", r)])
            p.act(Yb[r][:, :], Yb[r][:, :], AF.Exp, [("Y", r)], [("Y", r)], scale=-1.0, bias=LNS)
            p.tt("pool", qp[r][:, :], qT[:, tsl], Yb[r][:, :], ALU.mult, [("qT", g4), ("Y", r)], [("qp", r)])
            p.act(kw[r][:, :], psT[:, :], AF.Copy, ["psT", "w_tm"], [("kw", r)], scale=w_tm[:, c:c + 1])

        def s3():
            p.mm(UH[:, 0:256], kw[r][:, :], v_bf[:, c, :], True, True, [("kw", r), ("mv", g4)], [KUH])
            p.mm(UH[:, 256:384], kw[r][:, :], ones_bf[:, :], True, True, [("kw", r), "ones_bf"], [KUH])

        def s4():
            if c == 0:
                p.copy("dve", Cst[r][:, :], UH[:, 0:384], [KUH], [("Cst", r)])
            else:
                p.stt("dve", Cst[r][:, :], Cst[prev][:, :], dec[:, c:c + 1], UH[:, 0:384], ALU.mult, ALU.add,
                      [("Cst", prev), "dec", KUH], [("Cst", r)])
            p.copy("act", Cbf[r][:, :], Cst[r][:, :], [("Cst", r)], [("Cbf", r)])

        def s5():
            for hh in range(3):
                cs = slice(hh * 128, (hh + 1) * 128)
                lhs2 = v_bf[:, c, cs] if hh < 2 else ones_bf[:, :]
                if c > 0:
                    p.mm(UH[:, cs], Cbf[prev][:, cs], qp[r][:, :], True, False, [("Cbf", prev), ("qp", r), ("Cst", r)], [KUH])
                p.mm(UH[:, cs], lhs2, ST[r][:, :], c == 0, True, [("mv", g4), "ones_bf", ("ST", r), ("Cst", r)], [KUH])

        def s6():
            p.act(dab[r][:, :], UH[:, 256:384], AF.Abs, [KUH], [("dab", r)])
            p.tt("dve", dab[r][:, :], dab[r][:, :], AB[:, 128:256], ALU.max, [("dab", r), KAB], [("dab", r)])
            p.op("dve", lambda e: e.reciprocal(out=dab[r][:, :], in_=dab[r][:, :]), [("dab", r)], [("dab", r)])
            for hh in range(2):
                p.tt("dve", hout[r][:, hh, :], UH[:, hh * 128:(hh + 1) * 128], dab[r][:, :], ALU.mult,
                     [KUH, ("dab", r)], [("hout", r, hh)])
            p.dma("sp", d["hmT"].rearrange("(h p) t -> p h t", p=128)[:, :, tsl], hout[r][:, :, :], f"st_h{r}",
                  reads=[("hout", r, 0), ("hout", r, 1)], is_out=True)

        return [s1, s2, s3, s4, s5, s6]

    return chunk_stages, NT


def prep_B_mlstm(zf, zv, conv_w_l, gate_b_l):
    tri, maskp, ident = mlstm_consts()
    maps = []
    for j in range(NCORES):
        h, dr = j % 4, j // 4
        mqT = np.concatenate([zf[i][h * 128:(h + 1) * 128] for i in range(NCORES)], axis=1)
        mkT = np.concatenate([zf[i][512 + h * 128:512 + (h + 1) * 128] for i in range(NCORES)], axis=1)
        mv = np.concatenate([zv[i][:, h * 256:(h + 1) * 256] for i in range(NCORES)], axis=0)
        gi = np.concatenate([zf[i][3072 + (2 * dr) * 4 + h] for i in range(NCORES)], axis=0)
        gf = np.concatenate([zf[i][3072 + (2 * dr + 1) * 4 + h] for i in range(NCORES)], axis=0)
        cq = conv_w_l[:, h * 128:(h + 1) * 128]
        ck = conv_w_l[:, 512 + h * 128:512 + (h + 1) * 128]
        if dr == 1:
            mqT, mkT, mv, gi, gf = mqT[:, ::-1], mkT[:, ::-1], mv[::-1], gi[::-1], gf[::-1]
            cq, ck = cq[::-1], ck[::-1]
        gtm = np.concatenate([gi.reshape(64, 128).T, gf.reshape(64, 128).T], axis=1)
        mpar = np.concatenate([cq.T, ck.T, np.full((128, 1), gate_b_l[2 * dr, h], np.float32),
                               np.full((128, 1), gate_b_l[2 * dr + 1, h], np.float32)], axis=1)
        maps.append({"mqT": np.ascontiguousarray(mqT), "mkT": np.ascontiguousarray(mkT), "mv": np.ascontiguousarray(mv),
                     "gtm": np.ascontiguousarray(gtm, dtype=np.float32), "mpar": np.ascontiguousarray(mpar, dtype=np.float32),
                     "tri": tri, "maskp": maskp, "ident": ident})
    return maps


NR = 36


def build_C():
    p = Prog()
    T = TPC
    xT_d = p.dram("xT", [D, T], F32, "ExternalInput")
    par_d = p.dram("par", [128, 8 * KC], F32, "ExternalInput")
    wmo_d = p.dram("wmo", [8, 128, KC, 128], F32, "ExternalInput")
    wgm_d = p.dram("wgm", [16, 128, KC, 128], F32, "ExternalInput")
    wga_d = p.dram("wga", [16, 128, KC, 128], F32, "ExternalInput")
    wbm_d = p.dram("wbm", [16, 128, 8, 128], F32, "ExternalInput")
    wba_d = p.dram("wba", [16, 128, 8, 128], F32, "ExternalInput")
    wo_d = p.dram("wo", [16, 128, KC, 128], F32, "ExternalInput")
    hf_d = p.dram("hfT", [1024, T], F32, "ExternalInput")
    hb_d = p.dram("hbT", [1024, T], F32, "ExternalInput")
    ya_d = p.dram("yaT", [1024, T], F32, "ExternalInput")
    rw_d = p.dram("rw", [128, KC, NR], F32, "ExternalInput")
    rb_d = p.dram("rb", [128, 1], F32, "ExternalInput")
    ident_d = p.dram("ident", [128, 128], F32, "ExternalInput")
    iota_d = p.dram("iota", [128, 12], F32, "ExternalInput")
    xo_d = p.dram("xo", [D, T], F32, "ExternalOutput")
    h2_d = p.dram("h2T", [D, T], BF16, "ExternalOutput")
    rt_d = p.dram("rout", [T, 4], F32, "ExternalOutput")
    X = mybir.AxisListType.X

    par = p.sbuf([128, 8 * KC], F32)
    ones_f = p.sbuf([128, 128], F32)
    rw = p.sbuf([128, KC, NR], F32)
    rb = p.sbuf([128, 1], F32)
    ident = p.sbuf([128, 128], F32)
    iota = p.sbuf([128, 12], F32)
    merged = p.sbuf([128, KC, T], BF16)
    p.memset("pool", ones_f[:, :], 1.0, ["ones_f"])
    p.dma("sp", par[:, :], par_d[:, :], "ld_par", writes=["par"])
    p.dma("sp", rw[:, :, :], rw_d[:, :, :], "ld_rw", writes=["rw"])
    p.dma("sp", rb[:, :], rb_d[:, :], "ld_rb", writes=["rb"])
    p.dma("sp", ident[:, :], ident_d[:, :], "ld_ident", writes=["ident"])
    p.dma("sp", iota[:, :], iota_d[:, :], "ld_iota", writes=["iota"])
    ps_ss = [p.psum([128, 512]) for _ in range(2)]
    ps = [p.psum([128, 512]) for _ in range(6)]
    xv = xT_d.rearrange("(k p) t -> p k t", p=128)
    xr = [p.sbuf([128, T], F32) for _ in range(3)]
    xcnt = [0]

    def getx_stream(k, pass_no):
        r = xcnt[0] % 3
        xcnt[0] += 1
        p.dma("sp", xr[r][:, :], xv[:, k, :], f"ld_xr{r}", writes=[("xr", r)])
        return xr[r][:, :], ("xr", r)

    wring = [p.sbuf([128, KC, 128], BF16) for _ in range(4)]
    wcnt = [0]

    def loadw(src, nk=KC):
        r = wcnt[0] % 4
        wcnt[0] += 1
        p.dma("pool", wring[r][:, 0:nk, :], src, f"ld_w{r}", writes=[("w", r)])
        return wring[r], ("w", r)

    with p.scope():
        hT = p.sbuf([128, KC, T], BF16)
        ymT = p.sbuf([128, 8, T], BF16)
        yaT = p.sbuf([128, 8, T], BF16)
        _norm_mod(p, getx_stream, hT, par[:, 0:KC], par[:, KC:2 * KC], par[:, 2 * KC:3 * KC], ones_f, ps_ss, "par")
        hkeys = [("parh", k) for k in range(KC)]
        for g in range(2):
            p.dma("pool", yaT[:, 4 * g:4 * g + 4, :], ya_d.rearrange("(k p) t -> p k t", p=128)[:, 4 * g:4 * g + 4, :], "ld_ya",
                  writes=[("ya", g)])
        hs = [p.sbuf([128, 2, T], F32) for _ in range(2)]
        hb2 = [p.sbuf([128, 2, T], F32)] * 2
        hq = p.sbuf([128, T], F32)
        hr = p.sbuf([128, T], F32)
        sg = [p.sbuf([128, T], F32) for _ in range(2)]
        mi = 0
        for hd in range(4):
            r = hd % 2
            rows = slice(hd * 256, (hd + 1) * 256)
            p.dma("sp", hs[r][:, :, :], hf_d[rows, :].rearrange("(k p) t -> p k t", p=128), f"ld_hs{r}", writes=[("hs", r)])
            p.dma("sp", hb2[r][:, :, :], hb_d[rows, :].rearrange("(k p) t -> p k t", p=128), "ld_hb", writes=[("hb", 0)])
            p.tt("pool", hs[r][:, :, :], hs[r][:, :, :], hb2[r][:, :, :], ALU.add, [("hs", r), ("hb", 0)], [("hs", r)])
            for c2 in range(2):
                p.act(hq[:, :], hs[r][:, c2, :], AF.Square, [("hs", r)], ["hq"])
                for h in range(2):
                    p.mm(ps_ss[h][:, :], ones_f[:, :], hq[:, h * 512:(h + 1) * 512], c2 == 0, c2 == 1, ["ones_f", "hq"], [("parss", h)])
            for h in range(2):
                p.act(hr[:, h * 512:(h + 1) * 512], ps_ss[h][:, :], AF.Sqrt, [("parss", h)], ["hr"], bias=EPS, scale=1.0 / 256)
            p.op("dve", lambda e: e.reciprocal(out=hr[:, :], in_=hr[:, :]), ["hr"], ["hr"])
            for c2 in range(2):
                kb = hd * 2 + c2
                w, wk = loadw(wmo_d[kb])
                s_ = sg[kb % 2]
                for h in range(2):
                    pb = ps[mi % 6]
                    pk = ("ps", mi % 6)
                    mi += 1
                    for k in range(KC):
                        p.mm(pb[:, :], w[:, k, :], hT[:, k, h * 512:(h + 1) * 512], k == 0, k == KC - 1, [wk, hkeys[k]], [pk])
                    p.act(s_[:, h * 512:(h + 1) * 512], pb[:, :], AF.Sigmoid, [pk], [("sg", kb % 2)])
                p.stt("dve", hs[r][:, c2, :], hs[r][:, c2, :], par[:, 7 * KC + kb:7 * KC + kb + 1], hr[:, :], ALU.mult, ALU.mult,
                      [("hs", r), "hr", "par"], [("hs", r)])
                p.tt("dve", ymT[:, kb, :], hs[r][:, c2, :], s_[:, :], ALU.mult, [("hs", r), ("sg", kb % 2)], [("ym", kb)])
        t1 = [p.sbuf([128, 512], F32) for _ in range(2)]
        t2 = [p.sbuf([128, 512], F32) for _ in range(2)]
        s1 = [p.sbuf([128, 512], F32) for _ in range(2)]
        s2 = [p.sbuf([128, 512], F32) for _ in range(2)]
        it = 0
        for cb in range(16):
            wgm, kgm = loadw(wgm_d[cb])
            wga, kga = loadw(wga_d[cb])
            wbm, kbm = loadw(wbm_d[cb], 8)
            wba, kba = loadw(wba_d[cb], 8)
            for h in range(2):
                hsl = slice(h * 512, (h + 1) * 512)
                r = it % 2
                it += 1
                banks = []
                for w, wk, src, nk, skeys in ((wgm, kgm, hT, KC, hkeys), (wga, kga, hT, KC, hkeys),
                                              (wbm, kbm, ymT, 8, [("ym", k) for k in range(8)]),
                                              (wba, kba, yaT, 8, [("ya", k // 4) for k in range(8)])):
                    pb = ps[mi % 6]
                    pk = ("ps", mi % 6)
                    mi += 1
                    for k in range(nk):
                        p.mm(pb[:, :], w[:, k, :], src[:, k, hsl], k == 0, k == nk - 1, [wk, skeys[k]], [pk])
                    banks.append((pb, pk))
                p.act(s1[r][:, :], banks[0][0][:, :], AF.Sigmoid, [banks[0][1]], [("s1", r)])
                p.act(s2[r][:, :], banks[1][0][:, :], AF.Sigmoid, [banks[1][1]], [("s2", r)])
                p.tt("dve", t1[r][:, :], banks[2][0][:, :], s1[r][:, :], ALU.mult, [banks[2][1], ("s1", r)], [("t1", r)])
                p.tt("dve", t2[r][:, :], banks[3][0][:, :], s2[r][:, :], ALU.mult, [banks[3][1], ("s2", r)], [("t2", r)])
                p.tt("pool", merged[:, cb, hsl], t1[r][:, :], t2[r][:, :], ALU.add, [("t1", r), ("t2", r)], [("mg", cb)])

    with p.scope():
        xn = p.sbuf([128, KC, T], F32)
        h2T = p.sbuf([128, KC, T], BF16)
        mi = 0
        for cb in range(16):
            w, wk = loadw(wo_d[cb])
            r = xcnt[0] % 3
            xcnt[0] += 1
            p.dma("sp", xr[r][:, :], xv[:, cb, :], f"ld_xr{r}", writes=[("xr", r)])
            for h in range(2):
                hsl = slice(h * 512, (h + 1) * 512)
                pb = ps[mi % 4]
                pk = ("ps", mi % 4)
                mi += 1
                for k in range(KC):
                    p.mm(pb[:, :], w[:, k, :], merged[:, k, hsl], k == 0, k == KC - 1, [wk, ("mg", k)], [pk])
                p.stt("dve", xn[:, cb, hsl], pb[:, :], par[:, 3 * KC + cb:3 * KC + cb + 1], xr[r][:, hsl], ALU.mult, ALU.add,
                      [pk, "par", ("xr", r)], [("xn", cb, h)])
        for g in range(4):
            p.dma("sp", xo_d.rearrange("(k p) t -> p k t", p=128)[:, 4 * g:4 * g + 4, :], xn[:, 4 * g:4 * g + 4, :], f"st_x{g}",
                  reads=[("xn", k, h) for k in range(4 * g, 4 * g + 4) for h in range(2)], is_out=True)
        psr = ps[0]
        LT = p.sbuf([NR, T], F32)

        def router_hook(k, f, fkey):
            for h in range(2):
                p.mm(ps[4 + h][0:NR, :], rw[:, k, :], f[:, h * 512:(h + 1) * 512], k == 0, k == KC - 1, ["rw", fkey], [("ps", 4 + h)])

        class _XN:
            pass

        def getx_res(k, pass_no):
            return xn[:, k, :], ("xnall", k)

        for k in range(KC):
            p.op("pool", lambda e: e.engine_nop(), [("xn", k, 0), ("xn", k, 1)], [("xnall", k)])
        _norm_mod(p, getx_res, h2T, par[:, 4 * KC:5 * KC], par[:, 5 * KC:6 * KC], par[:, 6 * KC:7 * KC], ones_f, ps_ss, "par2",
                  hook=router_hook, tagpar="par")
        for g in range(4):
            p.dma("sp", h2_d.rearrange("(k p) t -> p k t", p=128)[:, 4 * g:4 * g + 4, :], h2T[:, 4 * g:4 * g + 4, :], f"st_h2{g}",
                  reads=[("par2h", k) for k in range(4 * g, 4 * g + 4)], is_out=True)
        NTT = T // 128
        for h in range(2):
            p.act(LT[:, h * 512:(h + 1) * 512], ps[4 + h][0:NR, :], AF.Identity, [("ps", 4 + h), "rb"], [("LT", h)], bias=rb[0:NR, 0:1])
        for tt_ in range(NTT):
            p.mm(psr[:, tt_ * NR:(tt_ + 1) * NR], LT[:, tt_ * 128:(tt_ + 1) * 128], ident[0:NR, 0:NR], True, True,
                 [("LT", tt_ // 4), "ident"], [("ps", 0)])
        L = p.sbuf([128, NTT, NR], F32)
        rout = p.sbuf([128, NTT, 4], F32)
        sc = p.sbuf([128, NTT, 16], F32)
        ohg = p.sbuf([128, NTT, 4], F32)
        ge = p.sbuf([128, NTT, 4], F32)
        wi = p.sbuf([128, NTT, 8], F32)
        wi2 = p.sbuf([128, NTT, 8], F32)
        oh1 = p.sbuf([128, NTT, 8], F32)
        oh2 = p.sbuf([128, NTT, 8], F32)
        tm8 = p.sbuf([128, NTT, 8], F32)
        for tt_ in range(NTT):
            K_ = ("rt", tt_)
            Lt = L[:, tt_, :]
            s = lambda j: sc[:, tt_, j:j + 1]
            p.copy("dve", Lt, psr[:, tt_ * NR:(tt_ + 1) * NR], [("ps", 0)], [K_])
            R_ = lambda fn: p.op("dve", fn, [K_, "iota"], [K_])
            R_(lambda e, Lt=Lt, tt_=tt_: e.reduce_max(out=sc[:, tt_, 0:1], in_=Lt[:, 0:4], axis=X))
            R_(lambda e, Lt=Lt, tt_=tt_: e.tensor_scalar(out=ge[:, tt_, :], in0=Lt[:, 0:4], scalar1=sc[:, tt_, 0:1], scalar2=None, op0=ALU.subtract))
            p.act(ge[:, tt_, :], ge[:, tt_, :], AF.Exp, [K_], [K_])
            R_(lambda e, tt_=tt_: e.reduce_sum(out=sc[:, tt_, 1:2], in_=ge[:, tt_, :], axis=X))
            R_(lambda e, tt_=tt_: e.reciprocal(out=sc[:, tt_, 2:3], in_=sc[:, tt_, 1:2]))
            R_(lambda e, Lt=Lt, tt_=tt_: e.tensor_scalar(out=ohg[:, tt_, :], in0=Lt[:, 0:4], scalar1=sc[:, tt_, 0:1], scalar2=None, op0=ALU.is_ge))
            R_(lambda e, Lt=Lt, tt_=tt_: e.tensor_scalar(out=wi[:, tt_, :], in0=Lt[:, 4:12], scalar1=ohg[:, tt_, 0:1], scalar2=None, op0=ALU.mult))
            for g in range(1, 4):
                R_(lambda e, Lt=Lt, tt_=tt_, g=g: e.scalar_tensor_tensor(out=wi[:, tt_, :], in0=Lt[:, 4 + 8 * g:12 + 8 * g], scalar=ohg[:, tt_, g:g + 1],
                                                                        in1=wi[:, tt_, :], op0=ALU.mult, op1=ALU.add))
            R_(lambda e, tt_=tt_: e.reduce_max(out=sc[:, tt_, 3:4], in_=wi[:, tt_, :], axis=X))
            R_(lambda e, tt_=tt_: e.tensor_scalar(out=oh1[:, tt_, :], in0=wi[:, tt_, :], scalar1=sc[:, tt_, 3:4], scalar2=None, op0=ALU.is_ge))
            R_(lambda e, tt_=tt_: e.scalar_tensor_tensor(out=wi2[:, tt_, :], in0=oh1[:, tt_, :], scalar=-1e30, in1=wi[:, tt_, :], op0=ALU.mult, op1=ALU.add))
            R_(lambda e, tt_=tt_: e.reduce_max(out=sc[:, tt_, 4:5], in_=wi2[:, tt_, :], axis=X))
            R_(lambda e, tt_=tt_: e.tensor_scalar(out=oh2[:, tt_, :], in0=wi2[:, tt_, :], scalar1=sc[:, tt_, 4:5], scalar2=None, op0=ALU.is_ge))
            R_(lambda e, tt_=tt_: e.tensor_tensor(out=sc[:, tt_, 5:6], in0=sc[:, tt_, 4:5], in1=sc[:, tt_, 3:4], op=ALU.subtract))
            p.act(sc[:, tt_, 6:7], sc[:, tt_, 5:6], AF.Exp, [K_], [K_])
            R_(lambda e, tt_=tt_: e.tensor_scalar(out=sc[:, tt_, 7:8], in0=sc[:, tt_, 6:7], scalar1=1.0, scalar2=None, op0=ALU.add))
            R_(lambda e, tt_=tt_: e.reciprocal(out=sc[:, tt_, 7:8], in_=sc[:, tt_, 7:8]))
            R_(lambda e, tt_=tt_: e.tensor_tensor(out=rout[:, tt_, 2:3], in0=sc[:, tt_, 7:8], in1=sc[:, tt_, 2:3], op=ALU.mult))
            R_(lambda e, tt_=tt_: e.tensor_tensor(out=rout[:, tt_, 3:4], in0=rout[:, tt_, 2:3], in1=sc[:, tt_, 6:7], op=ALU.mult))
            R_(lambda e, tt_=tt_: e.tensor_tensor(out=ge[:, tt_, :], in0=ohg[:, tt_, :], in1=iota[:, 8:12], op=ALU.mult))
            R_(lambda e, tt_=tt_: e.reduce_sum(out=sc[:, tt_, 8:9], in_=ge[:, tt_, :], axis=X))
            R_(lambda e, tt_=tt_: e.tensor_tensor(out=tm8[:, tt_, :], in0=oh1[:, tt_, :], in1=iota[:, 0:8], op=ALU.mult))
            R_(lambda e, tt_=tt_: e.reduce_sum(out=sc[:, tt_, 9:10], in_=tm8[:, tt_, :], axis=X))
            R_(lambda e, tt_=tt_: e.tensor_tensor(out=tm8[:, tt_, :], in0=oh2[:, tt_, :], in1=iota[:, 0:8], op=ALU.mult))
            R_(lambda e, tt_=tt_: e.reduce_sum(out=sc[:, tt_, 10:11], in_=tm8[:, tt_, :], axis=X))
            R_(lambda e, tt_=tt_: e.scalar_tensor_tensor(out=rout[:, tt_, 0:1], in0=sc[:, tt_, 8:9], scalar=8.0, in1=sc[:, tt_, 9:10], op0=ALU.mult, op1=ALU.add))
            R_(lambda e, tt_=tt_: e.scalar_tensor_tensor(out=rout[:, tt_, 1:2], in0=sc[:, tt_, 8:9], scalar=8.0, in1=sc[:, tt_, 10:11], op0=ALU.mult, op1=ALU.add))
        p.dma("sp", rt_d.rearrange("(t p) c -> p t c", p=128), rout[:, :, :], "st_rt", reads=[("rt", t) for t in range(NTT)], is_out=True)
    return p


def _blkN(w, c0, nblk, kc=KC):
    sub = w[:, c0:c0 + nblk * 128].reshape(kc, 128, nblk, 128)
    return np.ascontiguousarray(sub.transpose(2, 1, 0, 3))


def prep_C_weights(lw):
    w_in = lw["w_in"]
    out = {
        "wmo": _blkN(w_in, C_MO, 8), "wgm": _blkN(w_in, C_GM, 16), "wga": _blkN(w_in, C_GA, 16),
        "wbm": _blkN(lw["w_branch_m"], 0, 16, 8), "wba": _blkN(lw["w_branch_a"], 0, 16, 8), "wo": _blkN(lw["w_out"], 0, 16),
    }
    rwf = np.concatenate([lw["rg_w"], lw["re_w"]], axis=1)
    out["rw"] = np.ascontiguousarray(rwf.reshape(KC, 128, NR).transpose(1, 0, 2))
    rbc = np.zeros((128, 1), np.float32)
    rbc[:NR, 0] = np.concatenate([lw["rg_b"], lw["re_b"]])
    out["rb"] = rbc
    out["ident"] = np.eye(128, dtype=np.float32)
    out["iota"] = np.ascontiguousarray(np.tile(np.concatenate([np.arange(8), np.arange(4)])[None, :], (128, 1)).astype(np.float32))
    return out


def run_C(xT_sh, mod_l, lw, hmT, yaT):
    sh1, sc1, g1, sh2, sc2, g2 = [mod_l[i * D:(i + 1) * D] for i in range(6)]
    par = np.concatenate([_pk(lw["norm1_g"]), _pk(sc1), _pk(sh1), _pk(g1), _pk(lw["norm2_g"]), _pk(sc2), _pk(sh2),
                          _pk(lw["m_norm_g"]), np.zeros((128, 8), np.float32)], axis=1)
    cw = prep_C_weights(lw)
    in_maps = []
    for i in range(NCORES):
        ts = slice(i * TPC, (i + 1) * TPC)
        m = dict(cw)
        m["xT"] = xT_sh[i]
        m["par"] = par
        m["hfT"] = np.ascontiguousarray(np.concatenate([hmT[h][:, ts] for h in range(4)], axis=0))
        m["hbT"] = np.ascontiguousarray(np.concatenate([hmT[4 + h][:, ts] for h in range(4)], axis=0))
        m["yaT"] = np.ascontiguousarray(np.concatenate([yaT[j][:, ts] for j in range(8)], axis=0))
        in_maps.append(m)
    res = _run(build_C(), in_maps)
    return [r["xo"] for r in res], [r["h2T"] for r in res], [r["rout"] for r in res]


EPC = 4
DE = 512


def build_D(NJ, cap):
    p = Prog()
    hT_d = p.dram("hT", [NJ, D, cap], BF16, "ExternalInput")
    cw_d = p.dram("cw", [NJ, 128, cap // 128], F32, "ExternalInput")
    wg_d = p.dram("wg", [NJ, 128, KC, DE], F32, "ExternalInput")
    wu_d = p.dram("wu", [NJ, 128, KC, DE], F32, "ExternalInput")
    wd_d = p.dram("wd", [NJ, 128, 4, D], F32, "ExternalInput")
    y_d = p.dram("y", [NJ, cap, D], F32, "ExternalOutput")
    hsb = [p.sbuf([128, KC, cap], BF16) for _ in range(2)]
    cws = [p.sbuf([128, cap // 128], F32) for _ in range(2)]
    wg = [p.sbuf([128, KC, DE], BF16) for _ in range(2)]
    wu = [p.sbuf([128, KC, DE], BF16) for _ in range(2)]
    wd = [p.sbuf([128, 4, D], BF16) for _ in range(2)]
    aT = p.sbuf([128, 4, cap], BF16)
    sgb = [p.sbuf([128, 512], F32) for _ in range(2)]
    yst = [p.sbuf([128, D], F32) for _ in range(2)]
    ps = [p.psum([128, 512]) for _ in range(8)]
    mi = 0
    si = 0
    yi = 0
    for j in range(NJ):
        r = j % 2
        we = j % 2
        for g in range(4):
            p.dma("sp", hsb[r][:, 4 * g:4 * g + 4, :], hT_d[j].rearrange("(k p) t -> p k t", p=128)[:, 4 * g:4 * g + 4, :], f"ld_h{r}",
                  writes=[("h", r, g)])
        p.dma("sp", cws[r][:, :], cw_d[j], f"ld_cw{r}", writes=[("cw", r)])
        for g in range(4):
            p.dma("pool", wg[we][:, 4 * g:4 * g + 4, :], wg_d[j][:, 4 * g:4 * g + 4, :], f"ld_wg{we}", writes=[("wg", we, g)])
            p.dma("pool", wu[we][:, 4 * g:4 * g + 4, :], wu_d[j][:, 4 * g:4 * g + 4, :], f"ld_wu{we}", writes=[("wu", we, g)])
        for g in range(4):
            p.dma("pool", wd[we][:, g, :], wd_d[j][:, g, :], f"ld_wd{we}", writes=[("wd", we, g)])
        nb = (cap + 511) // 512
        for tb in range(nb):
            n0 = tb * 512
            n = min(512, cap - n0)
            for fb in range(4):
                pg = ps[mi % 8]
                kg = ("ps", mi % 8)
                mi += 1
                pu = ps[mi % 8]
                ku = ("ps", mi % 8)
                mi += 1
                for k in range(KC):
                    p.mm(pg[:, 0:n], wg[we][:, k, fb * 128:(fb + 1) * 128], hsb[r][:, k, n0:n0 + n], k == 0, k == KC - 1,
                         [("wg", we, k // 4), ("h", r, k // 4)], [kg])
                for k in range(KC):
                    p.mm(pu[:, 0:n], wu[we][:, k, fb * 128:(fb + 1) * 128], hsb[r][:, k, n0:n0 + n], k == 0, k == KC - 1,
                         [("wu", we, k // 4), ("h", r, k // 4)], [ku])
                s_ = sgb[si % 2]
                sk = ("sg", si % 2)
                si += 1
                p.act(s_[:, 0:n], pg[:, 0:n], AF.Silu, [kg], [sk])
                p.tt("dve", aT[:, fb, n0:n0 + n], pu[:, 0:n], s_[:, 0:n], ALU.mult, [ku, sk], [("aT", tb)])
        for tt_ in range(cap // 128):
            ys = yst[yi % 2]
            yk = ("yst", yi % 2)
            yi += 1
            for cb in range(4):
                py = ps[mi % 8]
                ky = ("ps", mi % 8)
                mi += 1
                for fb in range(4):
                    p.mm(py[:, :], aT[:, fb, tt_ * 128:(tt_ + 1) * 128], wd[we][:, fb, cb * 512:(cb + 1) * 512], fb == 0, fb == 3,
                         [("aT", tt_ // 4), ("wd", we, fb)], [ky])
                if cb % 2 == 0:
                    p.act(ys[:, cb * 512:(cb + 1) * 512], py[:, :], AF.Copy, [ky, ("cw", r)], [(yk, cb)], scale=cws[r][:, tt_:tt_ + 1])
                else:
                    p.ts("dve", ys[:, cb * 512:(cb + 1) * 512], py[:, :], cws[r][:, tt_:tt_ + 1], None, ALU.mult, None, [ky, ("cw", r)], [(yk, cb)])
            p.dma("sp", y_d[j][tt_ * 128:(tt_ + 1) * 128, :], ys[:, :], f"st_y{yi % 2}", reads=[(yk, cb) for cb in range(4)], is_out=True)
    return p


def _plan_D(counts):
    best = None
    for cap in (384, 512, 640, 768, 1024):
        nj = -(-int(sum(-(-int(c) // cap) for c in counts if c > 0)) // NCORES)
        nj = max(nj, 1)
        cost = nj * (70.0 + 0.08 * cap)
        if best is None or cost < best[0]:
            best = (cost, cap, nj)
    return best[1], best[2]


def run_D(h2T_sh, rout_sh, lw):
    h2 = np.concatenate([np.asarray(h).view(np.uint16).T for h in h2T_sh], axis=0)
    bf = np.asarray(h2T_sh[0]).dtype
    rout = np.concatenate(rout_sh, axis=0)
    ids = rout[:, 0:2].astype(np.int64)
    wts = rout[:, 2:4]
    toks, slots = [], []
    for e in range(32):
        t, s_ = np.nonzero(ids == e)
        toks.append(t)
        slots.append(s_)
    cap, NJ = _plan_D([len(t) for t in toks])
    jobs = []
    for e in range(32):
        for s0 in range(0, len(toks[e]), cap):
            jobs.append((e, toks[e][s0:s0 + cap], slots[e][s0:s0 + cap]))
    in_maps = []
    wgl = lw["e_w_gate"].reshape(32, KC, 128, DE)
    wul = lw["e_w_up"].reshape(32, KC, 128, DE)
    wdl = lw["e_w_down"].reshape(32, 4, 128, D)
    for i in range(NCORES):
        hT = np.zeros((NJ, D, cap), np.uint16)
        cw = np.zeros((NJ, 128, cap // 128), np.float32)
        wg = np.zeros((NJ, 128, KC, DE), np.float32)
        wu = np.zeros((NJ, 128, KC, DE), np.float32)
        wd = np.zeros((NJ, 128, 4, D), np.float32)
        for jl in range(NJ):
            jg = jl * NCORES + i
            if jg >= len(jobs):
                continue
            e, tt_, ss_ = jobs[jg]
            n = len(tt_)
            hT[jl][:, :n] = h2[tt_].T
            wfull = np.zeros(cap, np.float32)
            wfull[:n] = wts[tt_, ss_]
            cw[jl] = wfull.reshape(cap // 128, 128).T
            wg[jl] = wgl[e].transpose(1, 0, 2)
            wu[jl] = wul[e].transpose(1, 0, 2)
            wd[jl] = wdl[e].transpose(1, 0, 2)
        in_maps.append({"hT": hT.view(bf), "cw": cw, "wg": wg, "wu": wu, "wd": wd})
    res = _run(build_D(NJ, cap), in_maps)
    ys = np.zeros((2, S, D), np.float32)
    for i in range(NCORES):
        y = res[i]["y"]
        for jl in range(NJ):
            jg = jl * NCORES + i
            if jg >= len(jobs):
                continue
            e, tt_, ss_ = jobs[jg]
            ys[ss_, tt_] = y[jl][:len(tt_)]
    return [np.ascontiguousarray(ys[:, i * TPC:(i + 1) * TPC, :].transpose(0, 2, 1)) for i in range(NCORES)]


def build_E():
    p = Prog()
    T = TPC
    xT_d = p.dram("xT", [D, T], F32, "ExternalInput")
    yT_d = p.dram("yT", [2, D, T], F32, "ExternalInput")
    g_d = p.dram("g2", [128, KC], F32, "ExternalInput")
    xo_d = p.dram("xo", [D, T], F32, "ExternalOutput")
    g2 = p.sbuf([128, KC], F32)
    p.dma("sp", g2[:, :], g_d[:, :], "ld_g", writes=["g2"])
    xb = [p.sbuf([128, T], F32) for _ in range(3)]
    yb = [p.sbuf([128, 2, T], F32) for _ in range(3)]
    for k in range(KC):
        r = k % 3
        p.dma("sp", xb[r][:, :], xT_d.rearrange("(k p) t -> p k t", p=128)[:, k, :], f"ld_x{r}", writes=[("x", r)])
        p.dma("pool", yb[r][:, :, :], yT_d.rearrange("s (k p) t -> p k s t", p=128)[:, k, :, :], f"ld_y{r}", writes=[("y", r)])
        p.tt("pool", yb[r][:, 0, :], yb[r][:, 0, :], yb[r][:, 1, :], ALU.add, [("y", r)], [("y", r)])
        p.stt("dve", xb[r][:, :], yb[r][:, 0, :], g2[:, k:k + 1], xb[r][:, :], ALU.mult, ALU.add, [("y", r), ("x", r), "g2"], [("x", r)])
        p.dma("sp", xo_d.rearrange("(k p) t -> p k t", p=128)[:, k, :], xb[r][:, :], f"st_x{r}", reads=[("x", r)], is_out=True)
    return p


def run_E(xT_sh, yT_sh, g2):
    gp = _pk(g2)
    res = _run(build_E(), [{"xT": xT_sh[i], "yT": yT_sh[i], "g2": gp} for i in range(NCORES)])
    return [r["xo"] for r in res]


def run_B(zf, zv, lw, l):
    lam_init = 0.8 - 0.6 * math.exp(-0.3 * l)
    maps = prep_B_mlstm(zf, zv, lw["m_conv_w"], lw["m_gate_b"])
    cos16, sin16 = rope_tables_np()
    blk64, pm = const_mats()
    para = np.zeros((128, 8), np.float32)
    para[:, 0] = np.tile(lw["a_qnorm_g"], 2)
    para[:, 1] = np.tile(lw["a_knorm_g"], 2)
    para[:, 2] = lw["a_subln_g"]
    para[:, 3] = -lam_init
    para[:, 4] = 1.0 - lam_init
    lam_rep = np.ascontiguousarray(np.tile(lw["a_lambda"].reshape(1, 256), (128, 1)))
    for j in range(NCORES):
        m = maps[j]
        m["aqT"] = np.ascontiguousarray(np.concatenate([zf[i][1024 + j * 128:1024 + (j + 1) * 128] for i in range(NCORES)], axis=1))
        m["akT"] = np.ascontiguousarray(np.concatenate([zf[i][2048 + j * 128:2048 + (j + 1) * 128] for i in range(NCORES)], axis=1))
        m["av"] = np.ascontiguousarray(np.concatenate([zv[i][:, 1024 + j * 128:1024 + (j + 1) * 128] for i in range(NCORES)], axis=0))
        m.update({"para": para, "lam": lam_rep, "cos": cos16, "sin": sin16, "blk64": blk64, "pm": pm})
    res = _run(build_B(), maps)
    yaT = [r["yaT"] for r in res]
    hmT = [res[j]["hmT"] if j < 4 else np.ascontiguousarray(res[j]["hmT"][:, ::-1]) for j in range(NCORES)]
    return hmT, yaT


LAYER_KEYS = ["norm1_g", "norm2_g", "w_in", "m_conv_w", "m_gate_b", "m_norm_g", "a_qnorm_g", "a_knorm_g", "a_lambda", "a_subln_g",
              "w_branch_m", "w_branch_a", "w_out", "rg_w", "rg_b", "re_w", "re_b", "e_w_gate", "e_w_up", "e_w_down"]


def kernel(**inputs):
    x = np.asarray(inputs["x"], np.float32)[0]
    mod = run_mod(np.asarray(inputs["c"], np.float32), np.asarray(inputs["ada_w"], np.float32), np.asarray(inputs["ada_b"], np.float32))
    xT = [np.ascontiguousarray(x[i * TPC:(i + 1) * TPC].T) for i in range(NCORES)]
    yT = None
    g2prev = None
    for l in range(DEPTH):
        lw = {k: np.asarray(inputs[k][l], np.float32) for k in LAYER_KEYS}
        zf, zv, xT = run_A(xT, mod[l], lw["norm1_g"], lw["w_in"], g2prev, yT)
        hmT, yaT = run_B(zf, zv, lw, l)
        xT, h2T, rout = run_C(xT, mod[l], lw, hmT, yaT)
        yT = run_D(h2T, rout, lw)
        g2prev = mod[l][5 * D:6 * D]
    xo = run_E(xT, yT, g2prev)
    out = np.concatenate([o.T for o in xo], axis=0)[None]
    return np.ascontiguousarray(out.astype(np.float32))
```
